# Optimizing a Trainium2 kernel written in Bass

```python
import math
import jax
import jax.numpy as jnp
from jax import lax
import numpy as np

D_MODEL = 1024
BATCH = 8
SEQ = 2048
DEPTH = 4

MEM_LEN = 256
NORM_EPS = 1e-6
DOC_MEAN_LEN = 512
MASK_VALUE = -1e30
GATE_FLOOR = 1e-30

N_BRANCH = 4
BRANCH_W = D_MODEL // 4

MLA_HEADS = 4
MLA_Q_RANK = D_MODEL // 4
MLA_KV_RANK = D_MODEL // 8
MLA_NOPE = 64
MLA_ROPE = 32
MLA_V = BRANCH_W // MLA_HEADS
ROPE_THETA = 10000.0
Q_BLOCK = 128

HG_HEADS = 4
HG_DIM = BRANCH_W // HG_HEADS
HG_CHUNK = 16

ML_HEADS = 4
ML_DIM = BRANCH_W // ML_HEADS
ML_CONV = 4
ML_CHUNK = 64

S5_GROUP_CH = 16
S5_GROUPS = BRANCH_W // S5_GROUP_CH
S5_STATE = 64
S5_DT_MIN = 0.001
S5_DT_MAX = 0.1

XA_HEADS = 4
XA_DIM = D_MODEL // XA_HEADS

D_FF = ((8 * D_MODEL + 3 * 256 - 1) // (3 * 256)) * 256

SPLIT_SIZES = (MLA_Q_RANK, MLA_KV_RANK, MLA_ROPE,
               BRANCH_W, BRANCH_W, BRANCH_W, BRANCH_W,
               BRANCH_W, BRANCH_W, BRANCH_W, ML_HEADS, ML_HEADS,
               BRANCH_W,
               N_BRANCH * D_MODEL)
N_IN = sum(SPLIT_SIZES)

kernel_name = 'hybrid_mla_hgrn2_mlstm_s5_block'


def rms_norm(x, gain):
    xf = x.astype(jnp.float32)
    y = xf * lax.rsqrt(jnp.mean(xf * xf, axis=-1, keepdims=True) + NORM_EPS)
    return (y * gain.astype(jnp.float32)).astype(x.dtype)


def apply_rope(x, positions):
    half = x.shape[-1] // 2
    inv_freq = ROPE_THETA ** (-jnp.arange(half, dtype=jnp.float32) / half)
    ang = positions.astype(jnp.float32)[:, :, None, None] * inv_freq
    cos, sin = jnp.cos(ang), jnp.sin(ang)
    xf = x.astype(jnp.float32)
    x1, x2 = xf[..., :half], xf[..., half:]
    return jnp.concatenate([x1 * cos - x2 * sin, x2 * cos + x1 * sin], axis=-1).astype(x.dtype)


def causal_block_attention(q, k, v, scale):
    B, T, H, _ = q.shape
    nb = T // Q_BLOCK
    qb = q.reshape(B, nb, Q_BLOCK, H, -1).swapaxes(0, 1)
    kpos = jnp.arange(T)

    def one_block(args):
        qi, bi = args
        s = jnp.einsum('bqhd,bkhd->bhqk', qi, k).astype(jnp.float32) * scale
        qpos = bi * Q_BLOCK + jnp.arange(Q_BLOCK)
        s = jnp.where(kpos[None, :] <= qpos[:, None], s, MASK_VALUE)
        p = jax.nn.softmax(s, axis=-1).astype(v.dtype)
        return jnp.einsum('bhqk,bkhd->bqhd', p, v)

    out = lax.map(one_block, (qb, jnp.arange(nb)))
    return out.swapaxes(0, 1).reshape(B, T, H, -1)


def hgrn2_chunked(q, k, v, log_f):
    B, T, H, Dk = q.shape
    Dv = v.shape[-1]
    L = HG_CHUNK
    nc = T // L
    q, k, v, log_f = (a.reshape(B, nc, L, H, a.shape[-1]) for a in (q, k, v, log_f))
    b = jnp.cumsum(log_f, axis=2)
    b_end = b[:, :, -1]
    causal = jnp.tril(jnp.ones((L, L), dtype=bool))
    diff = b[:, :, :, None] - b[:, :, None]
    decay = jnp.exp(jnp.where(causal[None, None, :, :, None, None], diff, MASK_VALUE))
    scores = jnp.einsum('bcthd,bctshd,bcshd->bchts', q, decay, k)
    o_intra = jnp.einsum('bchts,bcshe->bcthe', scores, v)
    kv_end = jnp.einsum('bcshd,bcshe->bchde', k * jnp.exp(b_end[:, :, None] - b), v)

    def step(S, xs):
        dec, kv = xs
        return jnp.exp(dec)[..., None] * S + kv, S

    _, S0 = lax.scan(step, jnp.zeros((B, H, Dk, Dv), jnp.float32),
                     (b_end.swapaxes(0, 1), kv_end.swapaxes(0, 1)))
    S0 = S0.swapaxes(0, 1)
    o_inter = jnp.einsum('bcthd,bchde->bcthe', q * jnp.exp(b), S0)
    return (o_intra + o_inter).reshape(B, T, H, Dv)


def mlstm_chunked(q, k, v, i_log, f_log):
    B, T, H, Dk = q.shape
    Dv = v.shape[-1]
    L = ML_CHUNK
    nc = T // L
    q, k, v = (a.reshape(B, nc, L, H, a.shape[-1]) for a in (q, k, v))
    i_log = i_log.reshape(B, nc, L, H)
    b = jnp.cumsum(f_log.reshape(B, nc, L, H), axis=2)
    b_end = b[:, :, -1]
    g_end = b_end[:, :, None] - b + i_log

    def step(carry, xs):
        C, n, m = carry
        be, ge, kc, vc = xs
        m_new = jnp.maximum(be + m, ge.max(axis=1))
        keep = jnp.exp(be + m - m_new)
        w = jnp.exp(ge - m_new[:, None])
        C_new = keep[..., None, None] * C + jnp.einsum('blh,blhd,blhe->bhde', w, kc, vc)
        n_new = keep[..., None] * n + jnp.einsum('blh,blhd->bhd', w, kc)
        return (C_new, n_new, m_new), (C, n, m)

    init = (jnp.zeros((B, H, Dk, Dv), jnp.float32), jnp.zeros((B, H, Dk), jnp.float32),
            jnp.zeros((B, H), jnp.float32))
    _, (C0, n0, m0) = lax.scan(step, init, (b_end.swapaxes(0, 1), g_end.swapaxes(0, 1),
                                            k.swapaxes(0, 1), v.swapaxes(0, 1)))
    C0, n0, m0 = C0.swapaxes(0, 1), n0.swapaxes(0, 1), m0.swapaxes(0, 1)
    causal = jnp.tril(jnp.ones((L, L), dtype=bool))
    dmat = b[:, :, :, None, :] - b[:, :, None, :, :] + i_log[:, :, None, :, :]
    dmat = jnp.where(causal[None, None, :, :, None], dmat, MASK_VALUE)
    a = b + m0[:, :, None, :]
    m_t = jnp.maximum(a, dmat.max(axis=3))
    w_intra = jnp.exp(dmat - m_t[:, :, :, None, :])
    w_inter = jnp.exp(a - m_t)
    qk = jnp.einsum('bcthd,bcshd->bctsh', q, k) * w_intra
    num = (jnp.einsum('bctsh,bcshe->bcthe', qk, v)
           + w_inter[..., None] * jnp.einsum('bcthd,bchde->bcthe', q, C0))
    den = qk.sum(axis=3) + w_inter * jnp.einsum('bcthd,bchd->bcth', q, n0)
    h = num / jnp.maximum(jnp.abs(den), jnp.exp(-m_t))[..., None]
    return h.reshape(B, T, H, Dv)


def causal_depthwise_conv(x, w, b):
    y = lax.conv_general_dilated(x, w.astype(x.dtype)[:, None, :], window_strides=(1,),
                                 padding=((ML_CONV - 1, 0),),
                                 dimension_numbers=('NWC', 'WIO', 'NWC'),
                                 feature_group_count=x.shape[-1])
    return y + b.astype(x.dtype)


def _complex_linear_combine(e1, e2):
    ar1, ai1, br1, bi1 = e1
    ar2, ai2, br2, bi2 = e2
    return (ar2 * ar1 - ai2 * ai1, ar2 * ai1 + ai2 * ar1,
            ar2 * br1 - ai2 * bi1 + br2, ar2 * bi1 + ai2 * br1 + bi2)


def s5_ssm(u, a_re, a_im, log_dt, b_re, b_im, c_re, c_im, d):
    f32 = jnp.float32
    a_re, a_im = a_re.astype(f32), a_im.astype(f32)
    b_re, b_im, c_re, c_im = b_re.astype(f32), b_im.astype(f32), c_re.astype(f32), c_im.astype(f32)
    dt = jnp.exp(log_dt.astype(f32))[:, None]
    mag = jnp.exp(a_re * dt)
    ab_re, ab_im = mag * jnp.cos(a_im * dt), mag * jnp.sin(a_im * dt)
    inv_abs2 = 1.0 / (a_re * a_re + a_im * a_im)
    z_re = ((ab_re - 1.0) * a_re + ab_im * a_im) * inv_abs2
    z_im = (ab_im * a_re - (ab_re - 1.0) * a_im) * inv_abs2
    bb_re = z_re[..., None] * b_re - z_im[..., None] * b_im
    bb_im = z_re[..., None] * b_im + z_im[..., None] * b_re
    x_re = jnp.einsum('gph,btgh->btgp', bb_re, u)
    x_im = jnp.einsum('gph,btgh->btgp', bb_im, u)
    _, _, s_re, s_im = lax.associative_scan(
        _complex_linear_combine,
        (jnp.broadcast_to(ab_re, x_re.shape), jnp.broadcast_to(ab_im, x_im.shape), x_re, x_im),
        axis=1)
    return (jnp.einsum('ghp,btgp->btgh', c_re, s_re) - jnp.einsum('ghp,btgp->btgh', c_im, s_im)
            + d.astype(f32) * u)


def hybrid_token_mixer(h, positions, lower_bound, w_in, mla_q_norm, mla_w_uq, mla_kv_norm, mla_w_ukv,
                       hg_out_norm, ml_conv_w, ml_conv_b, ml_w_q, ml_w_k, ml_i_bias, ml_f_bias,
                       ml_out_norm, s5_a_re, s5_a_im, s5_log_dt, s5_b_re, s5_b_im, s5_c_re, s5_c_im,
                       s5_d, s5_w_glu, s5_b_glu, w_branch, w_out):
    B, T, _ = h.shape
    f32 = jnp.float32
    split_points = np.cumsum(SPLIT_SIZES)[:-1].tolist()
    (a_q, a_kv, a_kr, b_q, b_f, b_i, b_g, c_x, c_v, c_o, c_ig, c_fg, d_u,
     gate_logits) = jnp.split(h @ w_in, split_points, axis=-1)
    heads = lambda a, n: a.reshape(B, T, n, -1)

    q = (rms_norm(a_q, mla_q_norm) @ mla_w_uq).reshape(B, T, MLA_HEADS, MLA_NOPE + MLA_ROPE)
    q = jnp.concatenate([q[..., :MLA_NOPE], apply_rope(q[..., MLA_NOPE:], positions)], axis=-1)
    kv = (rms_norm(a_kv, mla_kv_norm) @ mla_w_ukv).reshape(B, T, MLA_HEADS, MLA_NOPE + MLA_V)
    k_rope = apply_rope(a_kr[:, :, None, :], positions)
    k = jnp.concatenate([kv[..., :MLA_NOPE],
                         jnp.broadcast_to(k_rope, (B, T, MLA_HEADS, MLA_ROPE))], axis=-1)
    out_a = causal_block_attention(q, k, kv[..., MLA_NOPE:],
                                   (MLA_NOPE + MLA_ROPE) ** -0.5).reshape(B, T, BRANCH_W)

    f_logit = b_f.astype(f32)
    lb = lower_bound.astype(f32)
    forget = lb + (1.0 - lb) * jax.nn.sigmoid(f_logit)
    log_f = jnp.log(jnp.maximum(forget, GATE_FLOOR))
    k_hg = (1.0 - lb) * jax.nn.sigmoid(-f_logit)
    o_hg = hgrn2_chunked(heads(jax.nn.silu(b_q.astype(f32)), HG_HEADS), heads(k_hg, HG_HEADS),
                         heads(b_i.astype(f32), HG_HEADS), heads(log_f, HG_HEADS))
    out_b = (rms_norm(o_hg, hg_out_norm.reshape(HG_HEADS, HG_DIM)).reshape(B, T, BRANCH_W)
             * jax.nn.silu(b_g.astype(f32)))

    xc = jax.nn.silu(causal_depthwise_conv(c_x, ml_conv_w, ml_conv_b)).reshape(B, T, ML_HEADS, ML_DIM)
    q_ml = jnp.einsum('bthd,hde->bthe', xc, ml_w_q).astype(f32)
    k_ml = jnp.einsum('bthd,hde->bthe', xc, ml_w_k).astype(f32) * ML_DIM ** -0.5
    i_log = c_ig.astype(f32) + ml_i_bias.astype(f32)
    f_log = jax.nn.log_sigmoid(c_fg.astype(f32) + ml_f_bias.astype(f32))
    h_ml = mlstm_chunked(q_ml, k_ml, heads(c_v.astype(f32), ML_HEADS), i_log, f_log)
    out_c = (jax.nn.sigmoid(c_o.astype(f32))
             * rms_norm(h_ml, ml_out_norm.reshape(ML_HEADS, ML_DIM)).reshape(B, T, BRANCH_W))

    u = d_u.astype(f32).reshape(B, T, S5_GROUPS, S5_GROUP_CH)
    y = jax.nn.gelu(s5_ssm(u, s5_a_re, s5_a_im, s5_log_dt, s5_b_re, s5_b_im, s5_c_re, s5_c_im,
                           s5_d).reshape(B, T, BRANCH_W))
    out_d = y * jax.nn.sigmoid(y @ s5_w_glu.astype(f32) + s5_b_glu.astype(f32))

    gates = jax.nn.sigmoid(gate_logits.astype(f32)).reshape(B, T, N_BRANCH, D_MODEL)
    branches = (out_a, out_b, out_c, out_d)
    merged = sum(gates[:, :, n] * (branches[n].astype(h.dtype) @ w_branch[n]) for n in range(N_BRANCH))
    return merged.astype(h.dtype) @ w_out


def memory_cross_attention(h, mem_n, wq, wk, wv, wo):
    B, T, _ = h.shape
    M = mem_n.shape[1]
    q = (h @ wq).reshape(B, T, XA_HEADS, XA_DIM)
    k = (mem_n @ wk).reshape(B, M, XA_HEADS, XA_DIM)
    v = (mem_n @ wv).reshape(B, M, XA_HEADS, XA_DIM)
    s = jnp.einsum('bthd,bmhd->bhtm', q, k).astype(jnp.float32) * XA_DIM ** -0.5
    p = jax.nn.softmax(s, axis=-1).astype(v.dtype)
    return jnp.einsum('bhtm,bmhd->bthd', p, v).reshape(B, T, D_MODEL) @ wo


def swiglu_ffn(h, w_in, w_out):
    a, b = jnp.split(h @ w_in, 2, axis=-1)
    return (jax.nn.silu(a) * b) @ w_out


def setup_inputs(seed: int = 0) -> dict:
    key = jax.random.key(seed)
    ks = iter(jax.random.split(key, 64))
    f32 = jnp.float32
    nrm = lambda shape, scale: scale * jax.random.normal(next(ks), shape, f32)
    gain = lambda shape: 1.0 + 0.1 * jax.random.normal(next(ks), shape, f32)
    L = DEPTH
    t = jnp.arange(SEQ, dtype=jnp.int32)
    starts = jax.random.bernoulli(next(ks), 1.0 / DOC_MEAN_LEN, (BATCH, SEQ)).at[:, 0].set(True)
    positions = (t - lax.cummax(jnp.where(starts, t, 0), axis=1)).astype(jnp.int32)
    return {
        'x': nrm((BATCH, SEQ, D_MODEL), 1.0),
        'mem': nrm((BATCH, MEM_LEN, D_MODEL), 1.0),
        'positions': positions,
        'norm_mix_pre': gain((L, D_MODEL)),
        'norm_mix_post': gain((L, D_MODEL)),
        'w_in': nrm((L, D_MODEL, N_IN), D_MODEL ** -0.5),
        'mla_q_norm': gain((L, MLA_Q_RANK)),
        'mla_w_uq': nrm((L, MLA_Q_RANK, MLA_HEADS * (MLA_NOPE + MLA_ROPE)), MLA_Q_RANK ** -0.5),
        'mla_kv_norm': gain((L, MLA_KV_RANK)),
        'mla_w_ukv': nrm((L, MLA_KV_RANK, MLA_HEADS * (MLA_NOPE + MLA_V)), MLA_KV_RANK ** -0.5),
        'hg_lb_logits': nrm((L, BRANCH_W), 0.1),
        'hg_out_norm': gain((L, BRANCH_W)),
        'ml_conv_w': nrm((L, ML_CONV, BRANCH_W), ML_CONV ** -0.5),
        'ml_conv_b': nrm((L, BRANCH_W), 0.01),
        'ml_w_q': nrm((L, ML_HEADS, ML_DIM, ML_DIM), ML_DIM ** -0.5),
        'ml_w_k': nrm((L, ML_HEADS, ML_DIM, ML_DIM), ML_DIM ** -0.5),
        'ml_i_bias': nrm((L, ML_HEADS), 0.1),
        'ml_f_bias': jnp.linspace(3.0, 6.0, ML_HEADS, dtype=f32)[None] + nrm((L, ML_HEADS), 0.1),
        'ml_out_norm': gain((L, BRANCH_W)),
        's5_a_re': -0.5 + nrm((L, S5_GROUPS, S5_STATE), 0.01),
        's5_a_im': jnp.pi * jnp.arange(S5_STATE, dtype=f32) + nrm((L, S5_GROUPS, S5_STATE), 0.01),
        's5_log_dt': jax.random.uniform(next(ks), (L, S5_GROUPS), f32,
                                        math.log(S5_DT_MIN), math.log(S5_DT_MAX)),
        's5_b_re': nrm((L, S5_GROUPS, S5_STATE, S5_GROUP_CH), (2 * S5_GROUP_CH) ** -0.5),
        's5_b_im': nrm((L, S5_GROUPS, S5_STATE, S5_GROUP_CH), (2 * S5_GROUP_CH) ** -0.5),
        's5_c_re': nrm((L, S5_GROUPS, S5_GROUP_CH, S5_STATE), S5_STATE ** -0.5),
        's5_c_im': nrm((L, S5_GROUPS, S5_GROUP_CH, S5_STATE), S5_STATE ** -0.5),
        's5_d': nrm((L, S5_GROUPS, S5_GROUP_CH), 1.0),
        's5_w_glu': nrm((L, BRANCH_W, BRANCH_W), BRANCH_W ** -0.5),
        's5_b_glu': nrm((L, BRANCH_W), 0.01),
        'w_branch': nrm((L, N_BRANCH, BRANCH_W, D_MODEL), BRANCH_W ** -0.5),
        'w_out': nrm((L, D_MODEL, D_MODEL), D_MODEL ** -0.5),
        'norm_xa_pre': gain((L, D_MODEL)),
        'norm_xa_post': gain((L, D_MODEL)),
        'norm_mem': gain((L, D_MODEL)),
        'xa_wq': nrm((L, D_MODEL, D_MODEL), D_MODEL ** -0.5),
        'xa_wk': nrm((L, D_MODEL, D_MODEL), D_MODEL ** -0.5),
        'xa_wv': nrm((L, D_MODEL, D_MODEL), D_MODEL ** -0.5),
        'xa_wo': nrm((L, D_MODEL, D_MODEL), D_MODEL ** -0.5),
        'norm_ffn_pre': gain((L, D_MODEL)),
        'norm_ffn_post': gain((L, D_MODEL)),
        'ffn_w_in': nrm((L, D_MODEL, 2 * D_FF), D_MODEL ** -0.5),
        'ffn_w_out': nrm((L, D_FF, D_MODEL), D_FF ** -0.5),
    }


def reference(x, mem, positions, norm_mix_pre, norm_mix_post, w_in, mla_q_norm, mla_w_uq,
              mla_kv_norm, mla_w_ukv, hg_lb_logits, hg_out_norm, ml_conv_w, ml_conv_b, ml_w_q,
              ml_w_k, ml_i_bias, ml_f_bias, ml_out_norm, s5_a_re, s5_a_im, s5_log_dt, s5_b_re,
              s5_b_im, s5_c_re, s5_c_im, s5_d, s5_w_glu, s5_b_glu, w_branch, w_out, norm_xa_pre,
              norm_xa_post, norm_mem, xa_wq, xa_wk, xa_wv, xa_wo, norm_ffn_pre, norm_ffn_post,
              ffn_w_in, ffn_w_out):
    lb_soft = jax.nn.softmax(hg_lb_logits.astype(jnp.float32), axis=0)
    lower_bounds = jnp.cumsum(lb_soft, axis=0) - lb_soft[0]
    for l in range(DEPTH):
        h = rms_norm(x, norm_mix_pre[l])
        mix = hybrid_token_mixer(h, positions, lower_bounds[l], w_in[l], mla_q_norm[l], mla_w_uq[l],
                                 mla_kv_norm[l], mla_w_ukv[l], hg_out_norm[l], ml_conv_w[l],
                                 ml_conv_b[l], ml_w_q[l], ml_w_k[l], ml_i_bias[l], ml_f_bias[l],
                                 ml_out_norm[l], s5_a_re[l], s5_a_im[l], s5_log_dt[l], s5_b_re[l],
                                 s5_b_im[l], s5_c_re[l], s5_c_im[l], s5_d[l], s5_w_glu[l],
                                 s5_b_glu[l], w_branch[l], w_out[l])
        x = x + rms_norm(mix, norm_mix_post[l])
        h = rms_norm(x, norm_xa_pre[l])
        xa = memory_cross_attention(h, rms_norm(mem, norm_mem[l]), xa_wq[l], xa_wk[l], xa_wv[l], xa_wo[l])
        x = x + rms_norm(xa, norm_xa_post[l])
        h = rms_norm(x, norm_ffn_pre[l])
        x = x + rms_norm(swiglu_ffn(h, ffn_w_in[l], ffn_w_out[l]), norm_ffn_post[l])
    return x
```

```python
import math
import numpy as np
from contextlib import ExitStack
import concourse.bass as bass
import concourse.mybir as mybir
from concourse.bass_utils import run_bass_kernel_spmd

F32 = mybir.dt.float32
BF16 = mybir.dt.bfloat16
I32 = mybir.dt.int32
AF = mybir.ActivationFunctionType
ALU = mybir.AluOpType
AX = mybir.AxisListType

T = 2048
D = 1024
NL = 4
MEM = 256
EPS = 1e-6
ENGS = ("pe", "act", "dve", "pool", "sp")


class Res:
    __slots__ = ("name", "w", "r", "sem", "pe_rows")

    def __init__(self, name=""):
        self.name = name
        self.w = None
        self.r = {}
        self.sem = None
        self.pe_rows = None


class Sched:
    def __init__(self, nc, es):
        self.nc = nc
        self.es = es
        self.q = {e: [] for e in ENGS}
        self.cnt = {}
        self.known = {e: {} for e in ENGS}
        self.sems = {}
        self.free_dma_sems = []
        self.dma_res = []
        for e in ENGS:
            self._sem("eng_" + e)
        self.n_dma_sems = 0

    def _sem(self, key):
        if key not in self.sems:
            self.sems[key] = self.es.enter_context(self.nc.semaphore(key))
            self.cnt[key] = 0
        return self.sems[key]

    def _deps(self, eng, reads, writes, pe_inorder=False):
        need = {}

        def add(s, n):
            if need.get(s, 0) < n:
                need[s] = n
        for r in reads:
            if r.w is not None:
                add(*r.w)
        for w in writes:
            if w.w is not None:
                add(*w.w)
            for s, n in w.r.items():
                add(s, n)
        waits = []
        kn = self.known[eng]
        if eng == "pe" and pe_inorder:
            need.pop("eng_pe", None)
        for s, n in need.items():
            if kn.get(s, 0) < n:
                waits.append((s, n))
                kn[s] = n
        return waits

    def _commit(self, ticket, reads, writes):
        s, n = ticket
        for r in reads:
            if r.r.get(s, 0) < n:
                r.r[s] = n
        for w in writes:
            w.w = ticket
            w.r = {}

    def op(self, eng, fn, reads=(), writes=(), pe_inorder=False):
        waits = self._deps(eng, reads, writes, pe_inorder)
        key = "eng_" + eng
        self.cnt[key] += 1
        ticket = (key, self.cnt[key])
        self.q[eng].append((waits, fn, (key, 1)))
        self._commit(ticket, reads, writes)
        return ticket

    def dma(self, eng, out, in_, reads=(), writes=(), **kw):
        tgt = writes[0] if writes else reads[0]
        if tgt.sem is None:
            if self.free_dma_sems:
                tgt.sem = self.free_dma_sems.pop()
            else:
                tgt.sem = "dma_%d" % self.n_dma_sems
                self.n_dma_sems += 1
            self.dma_res.append(tgt)
        semkey = tgt.sem
        self._sem(semkey)
        waits = self._deps(eng, reads, writes)
        self.cnt[semkey] += 16
        ticket = (semkey, self.cnt[semkey])
        self.q[eng].append((waits, lambda e: e.dma_start(out=out, in_=in_, **kw), (semkey, 16)))
        self._commit(ticket, reads, writes)
        return ticket

    def barrier(self, recycle=()):
        snap = [(k, self.cnt[k]) for k in self.sems if self.cnt[k] > 0]
        for e in ENGS:
            kn = self.known[e]
            waits = [(s, n) for s, n in snap if kn.get(s, 0) < n]
            for s, n in waits:
                kn[s] = n
            if waits:
                self.q[e].append((waits, None, None))
        for r in self.dma_res:
            if r.sem is not None:
                if r.sem not in self.free_dma_sems:
                    self.free_dma_sems.append(r.sem)
                r.sem = None
        self.dma_res = []

    def emit(self):
        nc = self.nc
        with nc.Block() as block:
            def replay(e, name):
                for waits, fn, inc in self.q[name]:
                    for s, n in waits:
                        e.wait_ge(self.sems[s], n)
                    if fn is None:
                        continue
                    ins = fn(e)
                    ins.then_inc(self.sems[inc[0]], inc[1])

            @block.tensor
            def _(e):
                replay(e, "pe")

            @block.scalar
            def _(e):
                replay(e, "act")

            @block.vector
            def _(e):
                replay(e, "dve")

            @block.gpsimd
            def _(e):
                replay(e, "pool")

            @block.sync
            def _(e):
                replay(e, "sp")


def _kmaj(w):
    K, N = w.shape
    return np.ascontiguousarray(w.reshape(K // 128, 128, N).transpose(1, 0, 2))


def _pp(v):
    return np.ascontiguousarray(v.reshape(-1, 128).T)


SMALL_COLS = {}
_off = 0
for _n, _w in [("g_mix_pre", 8), ("g_mix_post", 8), ("g_xa_pre", 8), ("g_xa_post", 8), ("g_mem", 8),
               ("g_ffn_pre", 8), ("g_ffn_post", 8), ("q_norm", 2), ("kv_norm", 1), ("hg_out_norm", 2),
               ("hg_lb_logits", 8), ("conv_w", 8), ("conv_b", 2), ("i_bias", 2), ("f_bias", 2),
               ("ml_out_norm", 2), ("b_glu", 2)]:
    SMALL_COLS[_n] = (_off, _w)
    _off += _w
NSMALL = _off


def prep_layer_small(inp, l):
    s = np.zeros((128, NSMALL), np.float32)

    def put(name, arr):
        o, w = SMALL_COLS[name]
        assert arr.shape == (128, w), (name, arr.shape)
        s[:, o:o + w] = arr
    for n, k in [("g_mix_pre", "norm_mix_pre"), ("g_mix_post", "norm_mix_post"), ("g_xa_pre", "norm_xa_pre"),
                 ("g_xa_post", "norm_xa_post"), ("g_mem", "norm_mem"), ("g_ffn_pre", "norm_ffn_pre"),
                 ("g_ffn_post", "norm_ffn_post")]:
        put(n, _pp(inp[k][l]))
    put("q_norm", _pp(inp["mla_q_norm"][l]))
    put("kv_norm", _pp(inp["mla_kv_norm"][l]))
    put("hg_out_norm", _pp(inp["hg_out_norm"][l]))
    lg = inp["hg_lb_logits"]
    put("hg_lb_logits", np.ascontiguousarray(lg.T.reshape(2, 128, NL).transpose(1, 0, 2).reshape(128, 8)))
    cw = inp["ml_conv_w"][l]
    put("conv_w", np.ascontiguousarray(cw.reshape(4, 2, 128).transpose(2, 0, 1).reshape(128, 8)))
    put("conv_b", _pp(inp["ml_conv_b"][l]))
    put("i_bias", _pp(np.repeat(inp["ml_i_bias"][l], 64)))
    put("f_bias", _pp(np.repeat(inp["ml_f_bias"][l], 64)))
    put("ml_out_norm", _pp(inp["ml_out_norm"][l]))
    put("b_glu", _pp(inp["s5_b_glu"][l]))
    return s


_SPL = np.cumsum([0, 256, 128, 32, 256, 256, 256, 256, 256, 256, 256, 4, 4, 256, 4096])
(C_AQ, C_AKV, C_AKR, C_BQ, C_BF, C_BI, C_BG, C_CX, C_CV, C_CO, C_CIG, C_CFG, C_DU, C_GATE, _) = [int(v) for v in _SPL]


def prep_weights(inp):
    W = {}
    w_in = inp["w_in"]
    perm32 = np.concatenate([np.arange(16, 32), np.arange(0, 16)])
    wa, wuq, wukv, wb, wc, wd, wg = [], [], [], [], [], [], []
    wbr, wout, xq, xk, xv, xo, f1, f2, mlq, mlk, wglu = [], [], [], [], [], [], [], [], [], [], []
    small = []
    s5p, s5d = [], []
    for l in range(NL):
        wi = w_in[l]
        akr = wi[:, C_AKR:C_AKR + 32]
        a = np.concatenate([wi[:, C_AQ:C_AQ + 256], wi[:, C_AKV:C_AKV + 128], akr, akr[:, perm32]], axis=1)
        wa.append(_kmaj(a))
        uq = inp["mla_w_uq"][l]
        uqp = np.concatenate([uq[:, h * 96 + 64:h * 96 + 96][:, perm32] for h in range(4)], axis=1)
        wuq.append(_kmaj(np.concatenate([uq, uqp], axis=1)))
        wukv.append(np.ascontiguousarray(inp["mla_w_ukv"][l]))
        wbk = _kmaj(wi[:, C_BQ:C_BQ + 1024]).reshape(128, 8, 4, 2, 128)
        wb.append(np.ascontiguousarray(wbk.transpose(3, 0, 1, 2, 4)))
        ig = np.repeat(wi[:, C_CIG:C_CIG + 4], 64, axis=1)
        fg = np.repeat(wi[:, C_CFG:C_CFG + 4], 64, axis=1)
        wck = _kmaj(np.concatenate([wi[:, C_CX:C_CX + 768], ig, fg], axis=1)).reshape(128, 8, 5, 2, 128)
        wc.append(np.ascontiguousarray(wck.transpose(3, 0, 1, 2, 4)))
        wd.append(_kmaj(wi[:, C_DU:C_DU + 256]))
        g = wi[:, C_GATE:C_GATE + 4096].reshape(8, 128, 4, 8, 128)
        wg.append(np.ascontiguousarray(g.transpose(3, 1, 2, 0, 4)))
        br = inp["w_branch"][l].reshape(4, 2, 128, 1024)
        wbr.append(np.ascontiguousarray(br.transpose(2, 0, 1, 3)))
        wout.append(_kmaj(inp["w_out"][l]))
        xq.append(_kmaj(inp["xa_wq"][l]))
        xk.append(_kmaj(inp["xa_wk"][l]))
        xv.append(_kmaj(inp["xa_wv"][l]))
        xo.append(_kmaj(inp["xa_wo"][l]))
        fi = inp["ffn_w_in"][l]
        pieces = [np.concatenate([fi[:, j * 128:(j + 1) * 128], fi[:, 2816 + j * 128:2816 + (j + 1) * 128]], axis=1)
                  for j in range(22)]
        f1.append(np.stack([_kmaj(p) for p in pieces]))
        fo = inp["ffn_w_out"][l].reshape(22, 128, 8, 128)
        f2.append(np.ascontiguousarray(fo.transpose(2, 1, 0, 3)))
        q_ = inp["ml_w_q"][l].reshape(2, 2, 64, 64)
        k_ = inp["ml_w_k"][l].reshape(2, 2, 64, 64)
        mlq.append(np.ascontiguousarray(q_.transpose(1, 2, 0, 3).reshape(128, 2, 64)))
        mlk.append(np.ascontiguousarray(k_.transpose(1, 2, 0, 3).reshape(128, 2, 64)))
        wglu.append(_kmaj(inp["s5_w_glu"][l]))
        def pl(a):
            sh = a.shape[2:]
            return np.ascontiguousarray(a.reshape(8, 2, 64, *sh).transpose(1, 2, 0, *range(3, 3 + len(sh))).reshape(128, 8, *sh))
        sp = np.zeros((128, 8, 67), np.float32)
        sp[:, :, 0] = pl(inp["s5_a_re"][l])
        sp[:, :, 1] = pl(inp["s5_a_im"][l])
        sp[:, :, 2] = pl(np.repeat(inp["s5_log_dt"][l][:, None], 64, axis=1))
        sp[:, :, 3:19] = pl(inp["s5_b_re"][l])
        sp[:, :, 19:35] = pl(inp["s5_b_im"][l])
        sp[:, :, 35:51] = pl(inp["s5_c_re"][l].transpose(0, 2, 1))
        sp[:, :, 51:67] = pl(inp["s5_c_im"][l].transpose(0, 2, 1))
        s5p.append(sp)
        s5d.append(np.repeat(inp["s5_d"][l].reshape(1, 256), 128, axis=0))
        small.append(prep_layer_small(inp, l))
    W["W_A"] = np.stack(wa)
    W["W_UQ"] = np.stack(wuq)
    W["W_UKV"] = np.stack(wukv)
    W["W_B"] = np.stack(wb)
    W["W_C"] = np.stack(wc)
    W["W_D"] = np.stack(wd)
    W["W_G"] = np.stack(wg)
    W["W_BR"] = np.stack(wbr)
    W["W_OUT"] = np.stack(wout)
    W["XA_Q"] = np.stack(xq)
    W["XA_K"] = np.stack(xk)
    W["XA_V"] = np.stack(xv)
    W["XA_O"] = np.stack(xo)
    W["W_F1"] = np.stack(f1)
    W["W_F2"] = np.stack(f2)
    W["ML_Q"] = np.stack(mlq)
    W["ML_K"] = np.stack(mlk)
    W["W_GLU"] = np.stack(wglu)
    W["SMALL"] = np.stack(small)
    W["S5P"] = np.stack(s5p)
    W["S5D"] = np.stack(s5d)
    return {k: np.ascontiguousarray(v, dtype=np.float32) for k, v in W.items()}


def make_consts():
    import ml_dtypes
    C = {}
    C["ident"] = np.eye(128, dtype=np.float32)
    C["ones"] = np.ones((128, 128), np.float32)
    bd = np.zeros((128, 128), np.float32)
    bd[:64, :64] = 1
    bd[64:, 64:] = 1
    C["ones_bd"] = bd
    s = np.arange(128)[:, None]
    t = np.arange(128)[None, :]
    C["negm"] = np.where(s <= t, 0.0, -30000.0).astype(np.float32)
    gm = ((s // 64 == t // 64) & (s <= t)).astype(np.float32)
    C["gla_mask"] = np.tile(gm, (1, 4))
    inv_freq = (10000.0 ** (-np.arange(16, dtype=np.float32) / 16)).astype(np.float32)
    ropec = np.zeros((128, 2), np.float32)
    ropec[64:96, 0] = np.concatenate([inv_freq, inv_freq])
    ropec[64:80, 1] = -1.0
    ropec[80:96, 1] = 1.0
    C["ropec"] = ropec
    rm = np.ones((128, T), np.float32)
    rm[:, ::64] = 0.0
    C["reset64"] = rm
    ev = np.concatenate([-np.arange(8), np.arange(8), np.arange(8) + 1, 7 - np.arange(8)]).astype(np.float32)
    C["s5_evec"] = np.tile(ev[None, :], (128, 1))
    C["s5_cidx"] = np.tile(np.arange(256, dtype=np.float32)[None, :], (128, 1))
    kk = np.arange(128)
    C["s5_mask4"] = np.tile(((kk[None, :] // 16) >= (kk[:, None] // 16)).astype(np.float32), (1, 4))
    cm = np.ones((128, 256), np.float32)
    cm[:, 0] = 0.0
    C["s5_cmask"] = cm
    return C


class KB:
    def __init__(self, nc, es, S, dram, taps):
        self.nc, self.es, self.S, self.dram = nc, es, S, dram
        self.taps = taps
        self.uid = 0
        self.ps = []
        for i in range(8):
            t = es.enter_context(nc.psum_tensor("ps%d" % i, [128, 512], F32))
            self.ps.append((t, Res("ps%d" % i)))
        self.ps_i = 0
        self.held = set()

    def psum(self):
        while True:
            i = self.ps_i
            self.ps_i = (self.ps_i + 1) % 8
            if i not in self.held:
                return self.ps[i]

    def psum_hold(self):
        while True:
            i = self.ps_i
            self.ps_i = (self.ps_i + 1) % 8
            if i not in self.held:
                self.held.add(i)
                return self.ps[i] + (i,)

    def psum_release(self, i):
        self.held.discard(i)

    def sb(self, stack, shape, dtype, name=None):
        self.uid += 1
        nm = "%s_%d" % (name or "t", self.uid)
        t = stack.enter_context(self.nc.sbuf_tensor(nm, list(shape), dtype))
        return t, Res(nm)

    def tap(self, name, ap, res, shape, dtype=F32):
        if name not in self.taps:
            return
        d = self.nc.dram_tensor("tap_" + name, list(shape), dtype, kind="ExternalOutput").ap()
        self.S.dma("sp", d, ap, reads=[res], writes=[Res("tap_" + name)])

    def mm(self, out, lhsT, rhs, start, stop, reads, writes):
        b0 = lhsT.base_partition()
        rows = (b0, b0 + lhsT.shape[0])
        w = writes[0]
        prev = w.pe_rows
        inorder = prev is None or not (rows[1] <= prev[0] or prev[1] <= rows[0])
        w.pe_rows = rows
        self.S.op("pe", lambda e: e.matmul(out, lhsT=lhsT, rhs=rhs, start=start, stop=stop), reads, writes, pe_inorder=inorder)

    def tr(self, out, in_, ident, reads, writes):
        self.S.op("pe", lambda e: e.transpose(out, in_, ident), reads, writes)

    def act(self, out, in_, func, reads, writes, **kw):
        self.S.op("act", lambda e: e.activation(out=out, in_=in_, func=func, **kw), reads, writes)

    def tt(self, out, in0, in1, op, reads, writes, eng="dve"):
        self.S.op(eng, lambda e: e.tensor_tensor(out=out, in0=in0, in1=in1, op=op), reads, writes)

    def tsc(self, out, in0, s1, s2, op0, op1, reads, writes, eng="dve"):
        if s2 is None:
            self.S.op(eng, lambda e: e.tensor_scalar(out=out, in0=in0, scalar1=s1, scalar2=None, op0=op0), reads, writes)
        else:
            self.S.op(eng, lambda e: e.tensor_scalar(out=out, in0=in0, scalar1=s1, scalar2=s2, op0=op0, op1=op1), reads, writes)

    def stt(self, out, in0, scalar, in1, op0, op1, reads, writes, eng="dve"):
        self.S.op(eng, lambda e: e.scalar_tensor_tensor(out=out, in0=in0, scalar=scalar, in1=in1, op0=op0, op1=op1), reads, writes)

    def cp(self, out, in_, reads, writes, eng="dve"):
        self.S.op(eng, lambda e: e.tensor_copy(out=out, in_=in_), reads, writes)

    def scan(self, out, d0, d1, init, op0, op1, reads, writes):
        self.S.op("dve", lambda e: e.tensor_tensor_scan(out=out, data0=d0, data1=d1, initial=init, op0=op0, op1=op1), reads, writes)

    def memset(self, ap, val, writes, eng="dve"):
        self.S.op(eng, lambda e: e.memset(ap, val), (), writes)

    def recip(self, out, in_, reads, writes):
        self.act(out, in_, AF.Ln, reads, writes)
        self.act(out, out, AF.Exp, writes, writes, scale=-1.0)

    def load_w(self, stack, dram_ap, shape, name="w", eng="pool"):
        t, r = self.sb(stack, shape, BF16, name)
        self.S.dma(eng, t[:], dram_ap, writes=[r])
        return t, r

    def rstd(self, out, sum_ps, nfeat, reads, Rout, nrows=128):
        self.act(out, sum_ps, AF.Ln, reads, [Rout], scale=1.0 / nfeat, bias=self.c_eps[0:nrows, :])
        self.act(out, out, AF.Exp, [Rout], [Rout], scale=-0.5)

    def setup(self, stack):
        S, dram = self.S, self.dram
        self.c_ident_f, _ = self.sb(stack, [128, 128], F32, "identf")
        self.c_ident, _ = self.sb(stack, [128, 128], BF16, "ident")
        self.c_ones, _ = self.sb(stack, [128, 128], BF16, "ones")
        self.c_ones_bd, _ = self.sb(stack, [128, 128], BF16, "onesbd")
        self.c_negm, _ = self.sb(stack, [128, 128], BF16, "negm")
        self.c_gla, _ = self.sb(stack, [128, 512], BF16, "glam")
        self.c_ropec, _ = self.sb(stack, [128, 2], F32, "ropec")
        self.c_eps, _ = self.sb(stack, [128, 1], F32, "eps")
        self.c_rm, _ = self.sb(stack, [128, 512], F32, "rm64")
        self.c_one, _ = self.sb(stack, [128, 1], F32, "one")
        self.small, _ = self.sb(stack, [128, NL, NSMALL], F32, "small")
        for dst, src, eng in [(self.c_ident_f, "ident", "sp"), (self.c_ident, "ident", "pool"),
                              (self.c_ones, "ones", "pool"), (self.c_ones_bd, "ones_bd", "pool"),
                              (self.c_negm, "negm", "pool"), (self.c_gla, "gla_mask", "pool"),
                              (self.c_ropec, "ropec", "sp")]:
            S.dma(eng, dst[:], dram[src], writes=[Res("c_" + src)])
        S.dma("act", self.c_rm[:], dram["reset64"][:, 0:512], writes=[Res("c_rm")])
        S.dma("sp", self.small[:], dram["SMALL"].rearrange("l p n -> p l n"), writes=[Res("c_small")])
        self.memset(self.c_eps[:], EPS, [Res("c_eps")])
        self.memset(self.c_one[:], 1.0, [Res("c_one")])
        self.xT, _ = self.sb(stack, [128, 8, T], F32, "xT")
        self.Rxb = [Res("x_tb%d" % i) for i in range(4)]
        for tb in range(4):
            S.dma("sp" if tb % 2 == 0 else "act", self.xT[:, :, tb * 512:(tb + 1) * 512],
                  dram["xT"][:, :, tb * 512:(tb + 1) * 512], writes=[self.Rxb[tb]])
        S.barrier()
        self.Rc = Res("consts")

    def sm(self, l, name, c=None, rows=slice(0, 128)):
        o, w = SMALL_COLS[name]
        if c is None:
            return self.small[rows, l, o:o + w]
        return self.small[rows, l, o + c:o + c + 1]

    def norm_fm(self, stack, src, src_rs, dst, dst_rs, l, gname, nK, nfeat, ntb=4, ncol=512):
        with ExitStack() as st:
            sq, _ = self.sb(st, [128, 2, ncol], BF16, "sq")
            Rsq2 = [Res("sq0"), Res("sq1")]
            rstd, Rrstd = self.sb(st, [128, ncol], F32, "rstd")
            tmp, Rtmp = self.sb(st, [128, ncol], F32, "tmp")
            for tb in range(ntb):
                ts = slice(tb * ncol, (tb + 1) * ncol)
                pt, pr = self.psum()
                for kt in range(nK):
                    self.act(sq[:, kt % 2, :], src[:, kt, ts], AF.Square, [src_rs[tb]], [Rsq2[kt % 2]])
                    self.mm(pt[:, 0:ncol], self.c_ones[:], sq[:, kt % 2, :], kt == 0, kt == nK - 1, [Rsq2[kt % 2]], [pr])
                self.rstd(rstd[:], pt[:, 0:ncol], nfeat, [pr], Rrstd)
                for kt in range(nK):
                    self.stt(dst[:, kt, ts], src[:, kt, ts], self.sm(l, gname, kt), rstd[:], ALU.mult, ALU.mult,
                             [src_rs[tb], Rrstd], [dst_rs[tb]])
            self.S.barrier()

    def postnorm_block(self, l, gname, y, Ry, tb, scr):
        sq, Rsq2, rstd, Rrstd, tmp, Rtmp = scr
        ts = slice(tb * 512, (tb + 1) * 512)
        pt, pr = self.psum()
        for m in range(8):
            self.act(sq[:, m % 2, :], y[:, m, :], AF.Square, [Ry], [Rsq2[m % 2]])
            self.mm(pt[:, :], self.c_ones[:], sq[:, m % 2, :], m == 0, m == 7, [Rsq2[m % 2]], [pr])
        self.rstd(rstd[:], pt[:, :], D, [pr], Rrstd)
        for m in range(8):
            self.tt(tmp[:], y[:, m, :], rstd[:], ALU.mult, [Ry, Rrstd], [Rtmp])
            self.stt(self.xT[:, m, ts], tmp[:], self.sm(l, gname, m), self.xT[:, m, ts], ALU.mult, ALU.add,
                     [Rtmp, self.Rxb[tb]], [self.Rxb[tb]])

    def postnorm_scratch(self, stack):
        sq, _ = self.sb(stack, [128, 2, 512], BF16, "psq")
        Rsq = [Res("psq0"), Res("psq1")]
        rstd, Rrstd = self.sb(stack, [128, 512], F32, "prstd")
        tmp, Rtmp = self.sb(stack, [128, 512], F32, "ptmp")
        return (sq, Rsq, rstd, Rrstd, tmp, Rtmp)

    def store_out(self):
        for tb in range(4):
            self.S.dma("sp" if tb % 2 == 0 else "act", self.dram["outT"][:, :, tb * 512:(tb + 1) * 512],
                       self.xT[:, :, tb * 512:(tb + 1) * 512], reads=[self.Rxb[tb]], writes=[Res("o%d" % tb)])
        self.S.barrier()

    def wslots(self, stack, shape, n, name="ws"):
        slots = [self.sb(stack, shape, BF16, name) for _ in range(n)]
        state = {"i": 0}

        def load(dram_ap, sub=None):
            t, r = slots[state["i"] % n]
            state["i"] += 1
            dst = t[:] if sub is None else sub(t)
            self.S.dma("pool", dst, dram_ap, writes=[r])
            return t, r
        return load

    def ffn(self, l):
        S, dram = self.S, self.dram
        with ExitStack() as st:
            hid, _ = self.sb(st, [128, 22, T], BF16, "hid")
            Rhid = [Res("hid%d" % i) for i in range(4)]
            with ExitStack() as st2:
                hT, _ = self.sb(st2, [128, 8, T], BF16, "hT")
                Rh = [Res("h%d" % i) for i in range(4)]
                self.norm_fm(st2, self.xT, self.Rxb, hT, Rh, l, "g_ffn_pre", 8, D)
                sil, Rsil = self.sb(st2, [128, 2, 512], F32, "sil")
                load1 = self.wslots(st2, [128, 8, 256], 2, "wf1")
                for j in range(22):
                    w, Rw = load1(dram["W_F1"][l, j])
                    for tb in range(4):
                        ts = slice(tb * 512, (tb + 1) * 512)
                        pa, ra = self.psum()
                        pb, rb = self.psum()
                        for kt in range(8):
                            self.mm(pa[:, :], w[:, kt, 0:128], hT[:, kt, ts], kt == 0, kt == 7, [Rw, Rh[tb]], [ra])
                        for kt in range(8):
                            self.mm(pb[:, :], w[:, kt, 128:256], hT[:, kt, ts], kt == 0, kt == 7, [Rw, Rh[tb]], [rb])
                        self.act(sil[:, tb % 2, :], pa[:, :], AF.Silu, [ra], [Rsil])
                        self.tt(hid[:, j, ts], sil[:, tb % 2, :], pb[:, :], ALU.mult, [Rsil, rb], [Rhid[tb]])
                S.barrier()
            with ExitStack() as st2:
                y2 = [self.sb(st2, [128, 8, 512], F32, "y") for _ in range(2)]
                scr = self.postnorm_scratch(st2)
                load2 = self.wslots(st2, [128, 22, 128], 2, "wf2")
                for half in range(2):
                    for m in range(8):
                        w, Rw = load2(dram["W_F2"][l, m])
                        for t2 in range(2):
                            tb = half * 2 + t2
                            ts = slice(tb * 512, (tb + 1) * 512)
                            y, Ry = y2[t2]
                            pt, pr = self.psum()
                            for kt in range(22):
                                self.mm(pt[:, :], w[:, kt, :], hid[:, kt, ts], kt == 0, kt == 21, [Rw, Rhid[tb]], [pr])
                            self.act(y[:, m, :], pt[:, :], AF.Copy, [pr], [Ry])
                    for t2 in range(2):
                        self.postnorm_block(l, "g_ffn_post", y2[t2][0], y2[t2][1], half * 2 + t2, scr)
                S.barrier()

    def xattn(self, l):
        S, dram = self.S, self.dram
        with ExitStack() as st:
            QT, _ = self.sb(st, [128, 8, T], BF16, "QT")
            RQ = [Res("q%d" % i) for i in range(4)]
            KT, RK = self.sb(st, [128, 8, MEM], BF16, "KT")
            V, RV = self.sb(st, [128, 2, D], BF16, "V")
            load = self.wslots(st, [128, 8, D], 2, "wxa")
            with ExitStack() as st2:
                memf, Rmemf = self.sb(st2, [128, 8, MEM], F32, "memf")
                memn, Rmemn = self.sb(st2, [128, 8, MEM], BF16, "memn")
                S.dma("sp", memf[:], dram["memT"], writes=[Rmemf])
                self.norm_fm(st2, memf, [Rmemf], memn, [Rmemn], l, "g_mem", 8, D, ntb=1, ncol=MEM)
                wk, Rwk = load(dram["XA_K"][l])
                for c in range(8):
                    pt, pr = self.psum()
                    for kt in range(8):
                        self.mm(pt[:, 0:MEM], wk[:, kt, c * 128:(c + 1) * 128], memn[:, kt, :], kt == 0, kt == 7, [Rwk, Rmemn], [pr])
                    self.act(KT[:, c, :], pt[:, 0:MEM], AF.Copy, [pr], [RK])
                wv, Rwv = load(dram["XA_V"][l])
                for mt in range(2):
                    for nb in range(2):
                        pt, pr = self.psum()
                        for kt in range(8):
                            self.mm(pt[:, :], memn[:, kt, mt * 128:(mt + 1) * 128], wv[:, kt, nb * 512:(nb + 1) * 512],
                                    kt == 0, kt == 7, [Rwv, Rmemn], [pr])
                        self.cp(V[:, mt, nb * 512:(nb + 1) * 512], pt[:, :], [pr], [RV])
                hT, _ = self.sb(st2, [128, 8, T], BF16, "hT")
                Rh = [Res("h%d" % i) for i in range(4)]
                self.norm_fm(st2, self.xT, self.Rxb, hT, Rh, l, "g_xa_pre", 8, D)
                wq, Rwq = load(dram["XA_Q"][l])
                for c in range(8):
                    for tb in range(4):
                        ts = slice(tb * 512, (tb + 1) * 512)
                        pt, pr = self.psum()
                        for kt in range(8):
                            self.mm(pt[:, :], wq[:, kt, c * 128:(c + 1) * 128], hT[:, kt, ts], kt == 0, kt == 7, [Rwq, Rh[tb]], [pr])
                        if (c + tb) % 2 == 0:
                            self.act(QT[:, c, ts], pt[:, :], AF.Copy, [pr], [RQ[tb]])
                        else:
                            self.cp(QT[:, c, ts], pt[:, :], [pr], [RQ[tb]])
                S.barrier()
            with ExitStack() as st2:
                AO, _ = self.sb(st2, [128, 8, T], BF16, "AO")
                RAO = [Res("ao%d" % i) for i in range(4)]
                P, _ = self.sb(st2, [128, 2, 2, 512], BF16, "P")
                RP = [Res("P0"), Res("P1")]
                rinv, Rrinv = self.sb(st2, [128, 512], F32, "rinv")
                wo, Rwo = load(dram["XA_O"][l])
                its = [(tb, h) for tb in range(4) for h in range(4)]

                def xscore(it):
                    tb, h = its[it]
                    ts = slice(tb * 512, (tb + 1) * 512)
                    for mt in range(2):
                        pt, pr = self.psum()
                        for dt in range(2):
                            self.mm(pt[:, :], KT[:, h * 2 + dt, mt * 128:(mt + 1) * 128], QT[:, h * 2 + dt, ts],
                                    dt == 0, dt == 1, [RK, RQ[tb]], [pr])
                        self.act(P[:, it % 2, mt, :], pt[:, :], AF.Exp, [pr], [RP[it % 2]], scale=1.0 / 16.0)

                xscore(0)
                for it, (tb, h) in enumerate(its):
                    ts = slice(tb * 512, (tb + 1) * 512)
                    if it + 1 < len(its):
                        xscore(it + 1)
                    Pi, RPi = P[:, it % 2], RP[it % 2]
                    pt, pr = self.psum()
                    for mt in range(2):
                        self.mm(pt[:, :], self.c_ones[:], Pi[:, mt, :], mt == 0, mt == 1, [RPi], [pr])
                    self.recip(rinv[:], pt[:, :], [pr], [Rrinv])
                    for et in range(2):
                        pt, pr = self.psum()
                        for mt in range(2):
                            self.mm(pt[:, :], V[:, mt, h * 256 + et * 128:h * 256 + (et + 1) * 128], Pi[:, mt, :],
                                    mt == 0, mt == 1, [RV, RPi], [pr])
                        self.tt(AO[:, h * 2 + et, ts], pt[:, :], rinv[:], ALU.mult, [pr, Rrinv], [RAO[tb]])
                ya, Rya = self.sb(st2, [128, 8, 512], F32, "y")
                yb = wq[:].bitcast(F32)
                y2 = [(ya, Rya), (yb, Rwq)]
                scr = self.postnorm_scratch(st2)
                def oproj(tb):
                    ts = slice(tb * 512, (tb + 1) * 512)
                    y, Ry = y2[tb % 2]
                    for m in range(8):
                        pt, pr = self.psum()
                        for kt in range(8):
                            self.mm(pt[:, :], wo[:, kt, m * 128:(m + 1) * 128], AO[:, kt, ts], kt == 0, kt == 7, [Rwo, RAO[tb]], [pr])
                        self.act(y[:, m, :], pt[:, :], AF.Copy, [pr], [Ry])

                oproj(0)
                for tb in range(4):
                    if tb + 1 < 4:
                        oproj(tb + 1)
                    self.postnorm_block(l, "g_xa_post", y2[tb % 2][0], y2[tb % 2][1], tb, scr)
                S.barrier()

    def rope_tables(self, stack):
        S = self.S
        cs, Rcs = self.sb(stack, [128, 2, T], F32, "cs")
        with ExitStack() as st:
            posi, Rpi = self.sb(st, [128, T], I32, "posi")
            ang, Ra = self.sb(st, [128, T], F32, "ang")
            kf, Rk = self.sb(st, [128, T], F32, "kf")
            ki, Rki = posi, Rpi
            rows = slice(64, 96)
            S.dma("sp", posi[rows, :], self.dram["pos"].partition_broadcast(32), writes=[Rpi])
            self.cp(ang[rows, :], posi[rows, :], [Rpi], [Ra])
            self.tsc(ang[rows, :], ang[rows, :], self.c_ropec[rows, 0:1], None, ALU.mult, None, [Ra], [Ra])
            for which in range(2):
                shift = math.pi / 2 if which == 0 else 0.0
                self.tsc(kf[rows, :], ang[rows, :], shift, 1.0 / (2 * math.pi), ALU.add, ALU.mult, [Ra], [Rk])
                self.cp(ki[rows, :], kf[rows, :], [Rk], [Rki])
                self.cp(kf[rows, :], ki[rows, :], [Rki], [Rk])
                c1 = 6.28125
                c2 = 2 * math.pi - c1
                dst = cs[rows, which, :]
                self.stt(dst, kf[rows, :], -c1, ang[rows, :], ALU.mult, ALU.add, [Rk, Ra], [Rcs])
                self.stt(dst, kf[rows, :], -c2, dst, ALU.mult, ALU.add, [Rk, Rcs], [Rcs])
                self.tsc(dst, dst, shift, 3.1415925, ALU.add, ALU.min, [Rcs], [Rcs])
                self.tsc(dst, dst, -3.1415925, None, ALU.max, None, [Rcs], [Rcs])
                self.act(dst, dst, AF.Sin, [Rcs], [Rcs])
            self.tsc(cs[rows, 1, :], cs[rows, 1, :], self.c_ropec[rows, 1:2], None, ALU.mult, None, [Rcs], [Rcs])
            S.barrier()
        return cs, Rcs

    def mla(self, l, hT, Rh, OUT, ROUT):
        S, dram = self.S, self.dram
        scale = 96 ** -0.5
        with ExitStack() as s1:
            wa, Rwa = self.load_w(s1, dram["W_A"][l], [128, 8, 448], "wa")
            wuq, Rwuq = self.load_w(s1, dram["W_UQ"][l], [128, 2, 512], "wuq")
            wukv, Rwukv = self.load_w(s1, dram["W_UKV"][l], [128, 512], "wukv")
            aqn, _ = self.sb(s1, [128, 2, T], BF16, "aqn")
            akvn, _ = self.sb(s1, [128, T], BF16, "akvn")
            krope, _ = self.sb(s1, [128, T], BF16, "krope")
            Raqn = [Res("aqn%d" % i) for i in range(4)]
            Rakvn = [Res("akvn%d" % i) for i in range(4)]
            Rkr = [Res("kr%d" % i) for i in range(4)]
            cs, Rcs = self.rope_tables(s1)
            rr = slice(64, 96)
            with ExitStack() as s2:
                af, Raf = self.sb(s2, [128, 3, 512], F32, "af")
                sq, Rsq = self.sb(s2, [128, 3, 512], BF16, "sq")
                rs, Rrs = self.sb(s2, [128, 2, 512], F32, "rs")
                t1, Rt1 = self.sb(s2, [128, 2, 512], F32, "t1")
                for tb in range(4):
                    ts = slice(tb * 512, (tb + 1) * 512)
                    for mt in range(3):
                        pt, pr = self.psum()
                        for kt in range(8):
                            self.mm(pt[:, :], wa[:, kt, mt * 128:(mt + 1) * 128], hT[:, kt, ts], kt == 0, kt == 7, [Rwa, Rh[tb]], [pr])
                        self.act(af[:, mt, :], pt[:, :], AF.Copy, [pr], [Raf])
                        self.act(sq[:, mt, :], pt[:, :], AF.Square, [pr], [Rsq])
                    pq, prq = self.psum()
                    self.mm(pq[:, :], self.c_ones[:], sq[:, 0, :], True, False, [Rsq], [prq])
                    self.mm(pq[:, :], self.c_ones[:], sq[:, 1, :], False, True, [Rsq], [prq])
                    self.rstd(rs[:, 0, :], pq[:, :], 256, [prq], Rrs)
                    pk, prk = self.psum()
                    self.mm(pk[:, :], self.c_ones[:], sq[:, 2, :], True, True, [Rsq], [prk])
                    self.rstd(rs[:, 1, :], pk[:, :], 128, [prk], Rrs)
                    for mt in range(2):
                        self.tt(t1[:, 0, :], af[:, mt, :], rs[:, 0, :], ALU.mult, [Raf, Rrs], [Rt1])
                        self.tsc(aqn[:, mt, ts], t1[:, 0, :], self.sm(l, "q_norm", mt), None, ALU.mult, None, [Rt1], [Raqn[tb]])
                    self.tt(t1[:, 1, :], af[:, 2, :], rs[:, 1, :], ALU.mult, [Raf, Rrs], [Rt1])
                    self.tsc(akvn[:, ts], t1[:, 1, :], self.sm(l, "kv_norm", 0), None, ALU.mult, None, [Rt1], [Rakvn[tb]])
                    pa, pra = self.psum()
                    pb, prb = self.psum()
                    for kt in range(8):
                        self.mm(pa[rr, :], wa[:, kt, 384:416], hT[:, kt, ts], kt == 0, kt == 7, [Rwa, Rh[tb]], [pra])
                    for kt in range(8):
                        self.mm(pb[rr, :], wa[:, kt, 416:448], hT[:, kt, ts], kt == 0, kt == 7, [Rwa, Rh[tb]], [prb])
                    self.tt(t1[rr, 0, :], pa[rr, :], cs[rr, 0, ts], ALU.mult, [pra, Rcs], [Rt1])
                    self.tt(t1[rr, 1, :], pb[rr, :], cs[rr, 1, ts], ALU.mult, [prb, Rcs], [Rt1])
                    self.tt(krope[rr, ts], t1[rr, 0, :], t1[rr, 1, :], ALU.add, [Rt1], [Rkr[tb]])
                S.barrier()
            for h in range(4):
                with ExitStack() as s2:
                    QT, _ = self.sb(s2, [128, T], BF16, "QT")
                    KT, _ = self.sb(s2, [128, T], BF16, "KT")
                    VP, RVP = self.sb(s2, [128, 16, 128], BF16, "VP")
                    RQ = [Res("Q%d" % i) for i in range(4)]
                    RK = [Res("K%d" % i) for i in range(4)]
                    t1, Rt1 = self.sb(s2, [128, 2, 512], F32, "t1")
                    rden, Rrden = self.sb(s2, [128, 512], F32, "rden")
                    odd = h % 2
                    vcol = slice(64, 128) if odd else slice(0, 64)
                    ocol = slice(0, 64) if odd else slice(64, 128)
                    self.memset(VP[:, :, ocol], 1.0, [RVP])
                    for tb in range(4):
                        ts = slice(tb * 512, (tb + 1) * 512)
                        qa, rqa = self.psum()
                        qb, rqb = self.psum()
                        for kt in range(2):
                            self.mm(qa[0:96, :], wuq[:, kt, h * 96:(h + 1) * 96], aqn[:, kt, ts], kt == 0, kt == 1, [Rwuq, Raqn[tb]], [rqa])
                        for kt in range(2):
                            self.mm(qb[rr, :], wuq[:, kt, 384 + h * 32:384 + (h + 1) * 32], aqn[:, kt, ts], kt == 0, kt == 1,
                                    [Rwuq, Raqn[tb]], [rqb])
                        self.act(QT[0:64, ts], qa[0:64, :], AF.Copy, [rqa], [RQ[tb]])
                        self.tt(t1[rr, 0, :], qa[rr, :], cs[rr, 0, ts], ALU.mult, [rqa, Rcs], [Rt1])
                        self.tt(t1[rr, 1, :], qb[rr, :], cs[rr, 1, ts], ALU.mult, [rqb, Rcs], [Rt1])
                        self.tt(QT[rr, ts], t1[rr, 0, :], t1[rr, 1, :], ALU.add, [Rt1], [RQ[tb]])
                        kn, rkn = self.psum()
                        self.mm(kn[0:64, :], wukv[:, h * 128:h * 128 + 64], akvn[:, ts], True, True, [Rwukv, Rakvn[tb]], [rkn])
                        self.act(KT[0:64, ts], kn[0:64, :], AF.Copy, [rkn], [RK[tb]])
                        self.act(KT[rr, ts], krope[rr, ts], AF.Copy, [Rkr[tb]], [RK[tb]])
                    for g8 in range(2):
                        pv, rpv = self.psum()
                        for j in range(8):
                            tt_ = g8 * 8 + j
                            self.mm(pv[:, j * 64:(j + 1) * 64], akvn[:, tt_ * 128:(tt_ + 1) * 128], wukv[:, h * 128 + 64:h * 128 + 128],
                                    True, True, [Rwukv, Rakvn[tt_ // 4]], [rpv])
                        self.cp(VP[:, g8 * 8:(g8 + 1) * 8, vcol], pv[:, :].rearrange("p (a b) -> p a b", b=64), [rpv], [RVP])
                    Pt4, _ = self.sb(s2, [128, 4, 512], BF16, "Pt4")
                    RP4 = [Res("P4_%d" % i) for i in range(4)]
                    for tb in range(4):
                        ts = slice(tb * 512, (tb + 1) * 512)
                        po, rpo, hold = self.psum_hold()
                        nst = 4 * tb + 4

                        def score(s_):
                            t0 = max(0, s_ * 128 - tb * 512)
                            N = 512 - t0
                            ps, rps = self.psum()
                            diag = s_ * 128 >= tb * 512
                            self.mm(ps[:, 0:N], KT[0:96, s_ * 128:(s_ + 1) * 128], QT[0:96, tb * 512 + t0:(tb + 1) * 512], True, not diag,
                                    [RK[s_ // 4], RQ[tb]], [rps])
                            if diag:
                                self.mm(ps[:, 0:128], self.c_ident[:], self.c_negm[:], False, True, [], [rps])
                            self.act(Pt4[:, s_ % 4, 0:N], ps[:, 0:N], AF.Exp, [rps], [RP4[s_ % 4]], scale=scale)

                        score(0)
                        for s_ in range(nst):
                            if s_ + 1 < nst:
                                score(s_ + 1)
                            t0 = max(0, s_ * 128 - tb * 512)
                            N = 512 - t0
                            self.mm(po[:, t0:512], VP[:, s_, :], Pt4[:, s_ % 4, 0:N], s_ == 0, s_ == nst - 1, [RVP, RP4[s_ % 4]], [rpo])
                        hs = slice(64, 128) if odd else slice(0, 64)
                        ds = slice(0, 64) if odd else slice(64, 128)
                        self.recip(rden[hs, :], po[ds, :], [rpo], [Rrden])
                        self.tt(OUT[hs, 0, h // 2, ts], po[hs, :], rden[hs, :], ALU.mult, [rpo, Rrden], [ROUT[0][tb]])
                        self.psum_release(hold)
                    S.barrier()
            S.barrier()

    def merge(self, l, hT, Rh, OUT, ROUT):
        S, dram = self.S, self.dram
        with ExitStack() as s1:
            merged, _ = self.sb(s1, [128, 8, T], BF16, "merged")
            Rmg = [Res("mg%d" % i) for i in range(4)]
            with ExitStack() as s2:
                wbr, Rwbr = self.load_w(s2, dram["W_BR"][l], [128, 4, 2, D], "wbr")
                load = self.wslots(s2, [128, 8, 128], 3, "wg")
                sig, Rsig = self.sb(s2, [128, 2, 512], F32, "sig")
                tmp, Rtmp = self.sb(s2, [128, 512], F32, "tmpm")
                k = 0
                for m in range(8):
                    for n in range(4):
                        wg, Rwg = load(dram["W_G"][l, m, :, n])
                        for tb in range(4):
                            ts = slice(tb * 512, (tb + 1) * 512)
                            pg, rpg = self.psum()
                            for kt in range(8):
                                self.mm(pg[:, :], wg[:, kt, :], hT[:, kt, ts], kt == 0, kt == 7, [Rwg, Rh[tb]], [rpg])
                            pb, rpb = self.psum()
                            for kt in range(2):
                                self.mm(pb[:, :], wbr[:, n, kt, m * 128:(m + 1) * 128], OUT[:, n, kt, ts], kt == 0, kt == 1,
                                        [Rwbr, ROUT[n][tb]], [rpb])
                            sg = sig[:, k % 2, :]
                            k += 1
                            self.act(sg, pg[:, :], AF.Sigmoid, [rpg], [Rsig])
                            if n == 0:
                                self.tt(self.macc[:, tb, :], sg, pb[:, :], ALU.mult, [Rsig, rpb], [self.Rmacc[tb]])
                            else:
                                self.tt(tmp[:], sg, pb[:, :], ALU.mult, [Rsig, rpb], [Rtmp])
                                if n < 3:
                                    self.tt(self.macc[:, tb, :], self.macc[:, tb, :], tmp[:], ALU.add, [Rtmp, self.Rmacc[tb]], [self.Rmacc[tb]])
                                else:
                                    self.tt(merged[:, m, ts], self.macc[:, tb, :], tmp[:], ALU.add, [Rtmp, self.Rmacc[tb]], [Rmg[tb]])
                S.barrier()
            S.barrier()
            wout = hT[:, 0:4, :].rearrange("p a (b c) -> p (a b) c", c=D)
            y = hT[:, 4:8, :].bitcast(F32).rearrange("p a (b c) -> p (a b) c", c=512)
            Rwout, Ry = Res("wout"), Res("y")
            S.dma("pool", wout, dram["W_OUT"][l], writes=[Rwout])
            o_sq = OUT[:, 0, 0, :].rearrange("p (a b) -> p a b", b=512)[:, 0:2, :]
            o_f = OUT[:, 1, :, :].bitcast(F32)
            scr = (o_sq, [Res("psq0"), Res("psq1")], o_f[:, 0, 0:512], Res("prstd"), o_f[:, 0, 512:1024], Res("ptmp"))
            yb = OUT[:, 2:4, :, :].bitcast(F32).rearrange("p a b (c d) -> p (a b c) d", d=512)
            y2 = [(y, Ry), (yb, Res("yb"))]
            def oproj(tb):
                ts = slice(tb * 512, (tb + 1) * 512)
                yy, Ryy = y2[tb % 2]
                for m in range(8):
                    pt, pr = self.psum()
                    for kt in range(8):
                        self.mm(pt[:, :], wout[:, kt, m * 128:(m + 1) * 128], merged[:, kt, ts], kt == 0, kt == 7, [Rwout, Rmg[tb]], [pr])
                    self.act(yy[:, m, :], pt[:, :], AF.Copy, [pr], [Ryy])

            oproj(0)
            for tb in range(4):
                if tb + 1 < 4:
                    oproj(tb + 1)
                self.postnorm_block(l, "g_mix_post", y2[tb % 2][0], y2[tb % 2][1], tb, scr)
            S.barrier()

    def mixer(self, l):
        with ExitStack() as st:
            hT, _ = self.sb(st, [128, 8, T], BF16, "hT")
            Rh = [Res("h%d" % i) for i in range(4)]
            OUT, _ = self.sb(st, [128, 4, 2, T], BF16, "OUT")
            ROUT = [[Res("out%d_%d" % (n, i)) for i in range(4)] for n in range(4)]
            self.norm_fm(st, self.xT, self.Rxb, hT, Rh, l, "g_mix_pre", 8, D)
            self.tap("hT", hT[:], Rh[3], [128, 8, T], BF16)
            if "a" in self.branches:
                self.mla(l, hT, Rh, OUT, ROUT)
            if "b" in self.branches:
                self.hgrn2(l, hT, Rh, OUT, ROUT)
            if "c" in self.branches:
                self.mlstm(l, hT, Rh, OUT, ROUT)
            if "d" in self.branches:
                self.s5(l, hT, Rh, OUT, ROUT)
            for n, nm in enumerate("abcd"):
                if nm not in self.branches:
                    for tb in range(4):
                        self.memset(OUT[:, n, :, tb * 512:(tb + 1) * 512], 0.0, [ROUT[n][tb]], eng="pool")
            self.tap("OUT", OUT[:], ROUT[0][3], [128, 4, 2, T], BF16)
            if "m" in self.branches:
                with ExitStack() as s2:
                    self.macc, _ = self.sb(s2, [128, 4, 512], F32, "macc")
                    self.Rmacc = [Res("macc%d" % i) for i in range(4)]
                    self.merge(l, hT, Rh, OUT, ROUT)
            self.S.barrier()

    def gla_pair(self, st, prep_alloc, prep_tb, VPp, RVP, EP, post_alloc, post, out_base_by_hh):
        S = self.S
        QK, _ = self.sb(st, [128, 4, T], BF16, "QK")
        qh, qt, kt_, kh = QK[:, 0, :], QK[:, 1, :], QK[:, 2, :], QK[:, 3, :]
        RQK = [Res("QK%d" % i) for i in range(4)]
        Rqh = Rqt = Rkt = Rkh = RQK
        dec, Rdec = self.sb(st, [128, 32], F32, "dec")
        with ExitStack() as s2:
            prep_alloc(s2)
            X, RX = self.sb(s2, [128, 5, 512], F32, "X")
            EX, REX = self.sb(s2, [128, 4, 512], F32, "EX")
            QKin, _ = self.sb(s2, [128, 2, 2, 512], F32, "QKin")
            RQKin = [Res("QKin0"), Res("QKin1")]
            for tb in range(4):
                ts = slice(tb * 512, (tb + 1) * 512)
                qk, Rqk = QKin[:, tb % 2], RQKin[tb % 2]
                lf, Rlf, ig, Rig = prep_tb(tb, qk[:, 0, :], qk[:, 1, :], Rqk)
                b = X[:, 0, :]
                self.scan(b, self.c_rm[:], lf, 0.0, ALU.mult, ALU.add, [Rlf], [RX])
                bch = b.rearrange("p (c j) -> p c j", j=64)
                ref = bch[:, :, 31:64:32].rearrange("p c r -> p r c").unsqueeze(3).to_broadcast([128, 2, 8, 64])
                self.tt(X[:, 1:3, :].rearrange("p r (c j) -> p r c j", j=64), bch.unsqueeze(1).to_broadcast([128, 2, 8, 64]), ref,
                        ALU.subtract, [RX], [RX])
                self.act(dec[:, tb * 8:(tb + 1) * 8], bch[:, :, 63], AF.Exp, [RX], [Rdec])
                if ig is not None:
                    self.tt(X[:, 3:5, :], ig.unsqueeze(1).to_broadcast([128, 2, 512]), X[:, 1:3, :], ALU.subtract, [Rig, RX], [RX])
                else:
                    self.tsc(X[:, 3:5, :], X[:, 1:3, :], -1.0, None, ALU.mult, None, [RX], [RX])
                self.act(EX[:, 0:2, :], X[:, 0:2, :], AF.Exp, [RX], [REX])
                self.act(EX[:, 2:4, :], X[:, 3:5, :], AF.Exp, [RX], [REX])
                self.tt(QK[:, :, ts].rearrange("p (a r) t -> p a r t", r=2), qk.unsqueeze(2).to_broadcast([128, 2, 2, 512]),
                        EX[:].rearrange("p (a r) t -> p a r t", r=2), ALU.mult, [Rqk, REX], [RQK[tb]])
            S.barrier()
        khT, RkhT = self.sb(st, [128, 16, 128], BF16, "khT")
        snap, _ = self.sb(st, [128, 32, EP], BF16, "snap")
        Rsnap = [Res("snap%d" % i) for i in range(32)]
        Sf2, _ = self.sb(st, [128, 2, EP], F32, "Sf")
        RSf2 = [Res("Sf0"), Res("Sf1")]
        Abf, _ = self.sb(st, [128, 4, 512], BF16, "Abf")
        RA = [Res("A%d" % i) for i in range(4)]
        post_alloc(st)
        for g8 in range(2):
            pt, pr = self.psum()
            ptb = pt[:, :].bitcast(BF16)
            for j in range(8):
                blk = g8 * 8 + j
                self.tr(ptb[:, j * 128:(j + 1) * 128], kh[:, blk * 128:(blk + 1) * 128], self.c_ident[:], [Rkh[blk // 4]], [pr])
            self.cp(khT[:, g8 * 8:(g8 + 1) * 8, :], ptb.rearrange("p (a b) -> p a b", b=128), [pr], [RkhT])
        self.memset(Sf2[:, 0, :], 0.0, [RSf2[0]])
        self.memset(snap[:, 0, :], 0.0, [Rsnap[0]])
        for c in range(31):
            blk, base = c // 2, (c % 2) * 64
            pk, rpk = self.psum()
            for hh in range(2):
                self.mm(pk[hh * 64:(hh + 1) * 64, 0:EP], khT[base:base + 64, blk, hh * 64:(hh + 1) * 64],
                        VPp[base:base + 64, blk, hh, :], True, True, [RkhT, RVP], [rpk])
            so, sn = Sf2[:, c % 2, :], Sf2[:, (c + 1) % 2, :]
            self.stt(sn, so, dec[:, c:c + 1], pk[:, 0:EP], ALU.mult, ALU.add, [RSf2[c % 2], Rdec, rpk], [RSf2[(c + 1) % 2]])
            self.act(snap[:, c + 1, :], sn, AF.Copy, [RSf2[(c + 1) % 2]], [Rsnap[c + 1]])
        NG = EP // 64

        def scores(tb):
            for hh in range(2):
                hs = slice(hh * 64, (hh + 1) * 64)
                pa, rpa = self.psum()
                for j in range(4):
                    blk = tb * 4 + j
                    bs = slice(blk * 128, (blk + 1) * 128)
                    self.mm(pa[:, j * 128:(j + 1) * 128], kt_[hs, bs], qt[hs, bs], True, True, [Rkt[tb], Rqt[tb]], [rpa])
                k_ = (tb % 2) * 2 + hh
                self.tt(Abf[:, k_, :], pa[:, :], self.c_gla[:], ALU.mult, [rpa], [RA[k_]])

        def values(tb):
            pos = []
            for g in range(NG):
                gc = slice(g * 64, (g + 1) * 64)
                po, rpo, hold = self.psum_hold()
                for hh in range(2):
                    hs = slice(hh * 64, (hh + 1) * 64)
                    k_ = (tb % 2) * 2 + hh
                    A, RAi = Abf[:, k_, :], RA[k_]
                    for j in range(4):
                        blk = tb * 4 + j
                        self.mm(po[hs, j * 128:(j + 1) * 128], VPp[:, blk, hh, gc], A[:, j * 128:(j + 1) * 128], True, False, [RVP, RAi], [rpo])
                        for cc in range(2):
                            c = blk * 2 + cc
                            cs_ = slice(blk * 128 + cc * 64, blk * 128 + (cc + 1) * 64)
                            self.mm(po[hs, j * 128 + cc * 64:j * 128 + (cc + 1) * 64], snap[hs, c, gc], qh[hs, cs_], False, cc == 1,
                                    [Rsnap[c], Rqh[tb]], [rpo])
                pos.append((po, rpo, hold))
            return pos

        scores(0)
        scores(1)
        pend = {0: values(0)}
        for tb in range(4):
            if tb + 2 < 4:
                scores(tb + 2)
            if tb + 1 < 4:
                pend[tb + 1] = values(tb + 1)
            pos = pend.pop(tb)
            post(tb, pos)
            for _, _, hold in pos:
                self.psum_release(hold)

    def headnorm_out(self, src, Rsrc, gain, gate, Rgate, out, Rout, scr):
        sq, Rsq, rs, Rrs, tmp, Rtmp = scr
        self.act(sq[:], src, AF.Square, [Rsrc], [Rsq])
        pt, pr = self.psum()
        self.mm(pt[:, :], self.c_ones_bd[:], sq[:], True, True, [Rsq], [pr])
        self.act(rs[:], pt[:, :], AF.Ln, [pr], [Rrs], scale=1.0 / 64, bias=self.c_eps[:])
        self.act(rs[:], rs[:], AF.Exp, [Rrs], [Rrs], scale=-0.5)
        self.tt(tmp[:], src, rs[:], ALU.mult, [Rsrc, Rrs], [Rtmp])
        self.stt(out, tmp[:], gain, gate, ALU.mult, ALU.mult, [Rtmp, Rgate], [Rout])

    def hgrn2(self, l, hT, Rh, OUT, ROUT):
        S, dram = self.S, self.dram
        with ExitStack() as s0:
            lbt, Rlb = self.sb(s0, [128, 2, 8], F32, "lbt")
            o, w = SMALL_COLS["hg_lb_logits"]
            lg = self.small[:, l, o:o + w].rearrange("p (t j) -> p t j", j=4)
            self.act(lbt[:, :, 0:4], lg, AF.Exp, [], [Rlb])
            for t_ in range(2):
                self.S.op("dve", lambda e, t_=t_: e.reduce_sum(out=lbt[:, t_, 4:5], in_=lbt[:, t_, 0:4], axis=AX.X), [Rlb], [Rlb])
                self.memset(lbt[:, t_, 5:6], 0.0, [Rlb])
                for j in range(1, l + 1):
                    self.tt(lbt[:, t_, 5:6], lbt[:, t_, 5:6], lbt[:, t_, j:j + 1], ALU.add, [Rlb], [Rlb])
                self.recip(lbt[:, t_, 4:5], lbt[:, t_, 4:5], [Rlb], [Rlb])
                self.tt(lbt[:, t_, 5:6], lbt[:, t_, 5:6], lbt[:, t_, 4:5], ALU.mult, [Rlb], [Rlb])
                self.tsc(lbt[:, t_, 6:7], lbt[:, t_, 5:6], -1.0, 1.0, ALU.mult, ALU.add, [Rlb], [Rlb])
                self.tsc(lbt[:, t_, 7:8], lbt[:, t_, 6:7], -1.0, None, ALU.mult, None, [Rlb], [Rlb])
            for pp in range(2):
                with ExitStack() as st:
                    wsrc = dram["W_B"][l, pp]
                    wb, Rwb = self.load_w(st, wsrc, [128, 8, 4, 128], "wb")
                    VPp, RVP = self.sb(st, [128, 16, 2, 64], BF16, "VPp")
                    Tm = {}

                    def prep_alloc(stk):
                        for nm in ("lf", "sg"):
                            Tm[nm] = self.sb(stk, [128, 512], F32, nm)

                    def post_alloc(stk):
                        Tm["gate"] = [self.sb(stk, [128, 512], BF16, "gate") for _ in range(4)]
                        Tm["scr"] = []
                        for _ in range(2):
                            sq, Rsq = self.sb(stk, [128, 512], BF16, "hsq")
                            rs, Rrs = self.sb(stk, [128, 512], F32, "hrs")
                            tmp, Rtmp = self.sb(stk, [128, 512], F32, "htmp")
                            Tm["scr"].append((sq, Rsq, rs, Rrs, tmp, Rtmp))
                        for tb in range(4):
                            gate, Rgate = Tm["gate"][tb]
                            pg, rg = proj(3, tb)
                            self.act(gate[:], pg[:, :], AF.Silu, [rg], [Rgate])
                    for g4 in range(4):
                        pv, rpv = self.psum()
                        for j in range(4):
                            blk = g4 * 4 + j
                            for kt in range(8):
                                self.mm(pv[:, j * 128:(j + 1) * 128], hT[:, kt, blk * 128:(blk + 1) * 128], wb[:, kt, 2, :], kt == 0, kt == 7,
                                        [Rwb, Rh[blk // 4]], [rpv])
                        self.cp(VPp[:, g4 * 4:(g4 + 1) * 4, :, :], pv[:, :].rearrange("p (a h e) -> p a h e", h=2, e=64), [rpv], [RVP])

                    def proj(a, tb):
                        pt, pr = self.psum()
                        for kt in range(8):
                            self.mm(pt[:, :], wb[:, kt, a, :], hT[:, kt, tb * 512:(tb + 1) * 512], kt == 0, kt == 7, [Rwb, Rh[tb]], [pr])
                        return pt, pr

                    def prep_tb(tb, qf, kf, Rqk):
                        (lf, Rlf), (sg, Rsg) = Tm["lf"], Tm["sg"]
                        pq, rq = proj(0, tb)
                        self.act(qf, pq[:, :], AF.Silu, [rq], [Rqk])
                        pf, rf = proj(1, tb)
                        self.act(sg[:], pf[:, :], AF.Sigmoid, [rf], [Rsg])
                        self.tsc(lf[:], sg[:], lbt[:, pp, 6:7], lbt[:, pp, 5:6], ALU.mult, ALU.add, [Rsg, Rlb], [Rlf])
                        self.tsc(lf[:], lf[:], 1e-30, None, ALU.max, None, [Rlf], [Rlf])
                        self.act(lf[:], lf[:], AF.Ln, [Rlf], [Rlf])
                        self.tsc(kf, sg[:], lbt[:, pp, 7:8], lbt[:, pp, 6:7], ALU.mult, ALU.add, [Rsg, Rlb], [Rqk])
                        return lf[:], Rlf, None, None

                    def post(tb, pos):
                        ts = slice(tb * 512, (tb + 1) * 512)
                        gate, Rgate = Tm["gate"][tb]
                        po, rpo, _ = pos[0]
                        self.headnorm_out(po[:, :], rpo, self.sm(l, "hg_out_norm", pp), gate[:], Rgate,
                                          OUT[:, 1, pp, ts], ROUT[1][tb], Tm["scr"][tb % 2])

                    self.gla_pair(st, prep_alloc, prep_tb, VPp, RVP, 64, post_alloc, post, lambda hh: slice(hh * 64, (hh + 1) * 64))
                    S.barrier()
            S.barrier()

    def mlstm(self, l, hT, Rh, OUT, ROUT):
        S, dram = self.S, self.dram
        for pp in range(2):
            with ExitStack() as st:
                wview = dram["W_C"][l, pp]
                mlq, Rmlq = self.load_w(st, dram["ML_Q"][l], [128, 2, 64], "mlq")
                mlk, Rmlk = self.load_w(st, dram["ML_K"][l], [128, 2, 64], "mlk")
                wc, Rwc = self.load_w(st, wview[:, :, 1:5, :], [128, 8, 4, 128], "wc")
                VPp, RVP = self.sb(st, [128, 16, 2, 128], BF16, "VPp")
                xc, _ = self.sb(st, [128, T], BF16, "xc")
                Rxc = [Res("xc%d" % i) for i in range(4)]
                nfb, Rnfb = self.sb(st, [128, 1], F32, "nfb")
                self.tsc(nfb[:], self.sm(l, "f_bias", pp), -1.0, None, ALU.mult, None, [], [Rnfb])
                self.memset(VPp[:, :, :, 64:128], 1.0, [RVP])

                def proj(w_ap, tb, reads):
                    pt, pr = self.psum()
                    for kt in range(8):
                        self.mm(pt[:, :], w_ap(kt), hT[:, kt, tb * 512:(tb + 1) * 512], kt == 0, kt == 7, reads + [Rh[tb]], [pr])
                    return pt, pr
                with ExitStack() as s2:
                    wx, Rwx = self.load_w(s2, wview[:, :, 0, :], [128, 8, 128], "wx")
                    cxf, Rcx = self.sb(s2, [128, 4 + T], F32, "cxf")
                    acc, Racc = self.sb(s2, [128, 512], F32, "acc")
                    self.memset(cxf[:, 0:4], 0.0, [Rcx])
                    for tb in range(4):
                        pt, pr = proj(lambda kt: wx[:, kt, :], tb, [Rwx])
                        self.act(cxf[:, 4 + tb * 512:4 + (tb + 1) * 512], pt[:, :], AF.Copy, [pr], [Rcx])
                    o, _w = SMALL_COLS["conv_w"]
                    for tb in range(4):
                        base = 4 + tb * 512
                        cw = lambda j: self.small[:, l, o + j * 2 + pp:o + j * 2 + pp + 1]
                        self.tsc(acc[:], cxf[:, base:base + 512], cw(3), None, ALU.mult, None, [Rcx], [Racc])
                        for j in range(3):
                            sh = 3 - j
                            self.stt(acc[:], cxf[:, base - sh:base - sh + 512], cw(j), acc[:], ALU.mult, ALU.add, [Rcx, Racc], [Racc])
                        self.act(xc[:, tb * 512:(tb + 1) * 512], acc[:], AF.Silu, [Racc], [Rxc[tb]], bias=self.sm(l, "conv_b", pp))
                    S.barrier()
                for g4 in range(4):
                    pv, rpv = self.psum()
                    for j in range(4):
                        blk = g4 * 4 + j
                        for kt in range(8):
                            self.mm(pv[:, j * 128:(j + 1) * 128], hT[:, kt, blk * 128:(blk + 1) * 128], wc[:, kt, 0, :], kt == 0, kt == 7,
                                    [Rwc, Rh[blk // 4]], [rpv])
                    pvv = pv[:, :].rearrange("p (a h e) -> p a h e", h=2, e=64)
                    self.cp(VPp[:, g4 * 4:(g4 + 1) * 4, :, 0:64], pvv, [rpv], [RVP])
                Tm = {}

                def prep_alloc(stk):
                    for nm in ("lf", "ig", "e1"):
                        Tm[nm] = self.sb(stk, [128, 512], F32, nm)

                def post_alloc(stk):
                    Tm["gate"] = [self.sb(stk, [128, 512], BF16, "gate") for _ in range(4)]
                    Tm["aden"] = [self.sb(stk, [128, 512], F32, "aden") for _ in range(2)]
                    Tm["hml"] = [self.sb(stk, [128, 512], F32, "hml")] * 2
                    Tm["scr"] = []
                    for _ in range(2):
                        sq, Rsq = self.sb(stk, [128, 512], BF16, "hsq")
                        rs, Rrs = self.sb(stk, [128, 512], F32, "hrs")
                        tmp, Rtmp = self.sb(stk, [128, 512], F32, "htmp")
                        Tm["scr"].append((sq, Rsq, rs, Rrs, tmp, Rtmp))
                    for tb in range(4):
                        gate, Rgate = Tm["gate"][tb]
                        pg, rg = proj(lambda kt: wc[:, kt, 1, :], tb, [Rwc])
                        self.act(gate[:], pg[:, :], AF.Sigmoid, [rg], [Rgate])

                def prep_tb(tb, qf, kf, Rqk):
                    (lf, Rlf), (ig, Rig), (e1, Re1) = Tm["lf"], Tm["ig"], Tm["e1"]
                    ts = slice(tb * 512, (tb + 1) * 512)
                    pq, rq = self.psum()
                    pk, rk = self.psum()
                    for hh in range(2):
                        hs = slice(hh * 64, (hh + 1) * 64)
                        self.mm(pq[hs, :], mlq[hs, pp, :], xc[hs, ts], True, True, [Rmlq, Rxc[tb]], [rq])
                        self.mm(pk[hs, :], mlk[hs, pp, :], xc[hs, ts], True, True, [Rmlk, Rxc[tb]], [rk])
                    self.act(qf, pq[:, :], AF.Copy, [rq], [Rqk])
                    self.act(kf, pk[:, :], AF.Copy, [rk], [Rqk], scale=0.125)
                    pf, rf = proj(lambda kt: wc[:, kt, 3, :], tb, [Rwc])
                    self.act(e1[:], pf[:, :], AF.Exp, [rf, Rnfb], [Re1], scale=-1.0, bias=nfb[:])
                    self.act(e1[:], e1[:], AF.Ln, [Re1], [Re1], bias=self.c_one[:])
                    self.tsc(lf[:], e1[:], -1.0, None, ALU.mult, None, [Re1], [Rlf])
                    pi_, ri = proj(lambda kt: wc[:, kt, 2, :], tb, [Rwc])
                    self.act(ig[:], pi_[:, :], AF.Identity, [ri], [Rig], bias=self.sm(l, "i_bias", pp))
                    return lf[:], Rlf, ig[:], Rig

                def post(tb, pos):
                    ts = slice(tb * 512, (tb + 1) * 512)
                    (gate, Rgate), (aden, Raden), (hml, Rhml) = Tm["gate"][tb], Tm["aden"][tb % 2], Tm["hml"][tb % 2]
                    (pn, rpn, _), (pd, rpd, _) = pos
                    self.act(aden[:], pd[:, :], AF.Abs, [rpd], [Raden])
                    self.tsc(aden[:], aden[:], 1.0, None, ALU.max, None, [Raden], [Raden])
                    self.recip(aden[:], aden[:], [Raden], [Raden])
                    self.tt(hml[:], pn[:, :], aden[:], ALU.mult, [rpn, Raden], [Rhml])
                    self.headnorm_out(hml[:], Rhml, self.sm(l, "ml_out_norm", pp), gate[:], Rgate,
                                      OUT[:, 2, pp, ts], ROUT[2][tb], Tm["scr"][tb % 2])

                self.gla_pair(st, prep_alloc, prep_tb, VPp, RVP, 128, post_alloc, post, lambda hh: slice(0, 128))
                S.barrier()
        S.barrier()

    def sin_of(self, dst, ang, shift, kf, ki, R):
        c1 = 6.28125
        c2 = 2 * math.pi - c1
        self.tsc(kf, ang, shift, 1.0 / (2 * math.pi), ALU.add, ALU.mult, [R], [R])
        self.cp(ki, kf, [R], [R])
        self.cp(kf, ki, [R], [R])
        self.stt(dst, kf, -c1, ang, ALU.mult, ALU.add, [R], [R])
        self.stt(dst, kf, -c2, dst, ALU.mult, ALU.add, [R], [R])
        self.tsc(dst, dst, shift, 3.1415925, ALU.add, ALU.min, [R], [R])
        self.tsc(dst, dst, -3.1415925, None, ALU.max, None, [R], [R])
        self.act(dst, dst, AF.Sin, [R], [R])

    def s5(self, l, hT, Rh, OUT, ROUT):
        S, dram = self.S, self.dram
        M, A_, SUB = ALU.mult, ALU.add, ALU.subtract
        with ExitStack() as st:
            Tg, RTg = self.sb(st, [128, 16, 128], BF16, "Tg")
            G, RG = self.sb(st, [128, 16, 2, 64], BF16, "G")
            Rpr, RRp = self.sb(st, [128, 8, 128], BF16, "Rpr")
            Rpi, _ = self.sb(st, [128, 8, 128], BF16, "Rpi")
            rho, Rp = self.sb(st, [128, 8], F32, "rho")
            phi, _ = self.sb(st, [128, 8], F32, "phi")
            with ExitStack() as s2:
                prm, _ = self.sb(s2, [128, 8, 67], F32, "prm")
                ev, _ = self.sb(s2, [128, 32], F32, "ev")
                msk, _ = self.sb(s2, [128, 512], F32, "msk")
                S.dma("sp", prm[:], dram["S5P"][l], writes=[Res("prm")])
                S.dma("sp", ev[:], dram["s5_evec"], writes=[Res("ev")])
                S.dma("sp", msk[:], dram["s5_mask4"], writes=[Res("msk")])
                S.barrier()
                sm_ = lambda n: self.sb(s2, [128, 8], F32, n)[0]
                dt, adt, th, t8, k8, lm1, den, zr, zi, ta, tb_ = [sm_(n) for n in "dt adt th t8 k8 lm1 den zr zi ta tb".split()]
                k8i, _ = self.sb(s2, [128, 8], I32, "k8i")
                big = lambda n: self.sb(s2, [128, 8, 32], F32, n)[0]
                angE, magE, cosE, sinE, Pr, Pi, kfE = [big(n) for n in "angE magE cosE sinE Pr Pi kfE".split()]
                kiE, _ = self.sb(s2, [128, 8, 32], I32, "kiE")
                R = [Rp]
                ar, ai = prm[:, :, 0], prm[:, :, 1]
                self.act(dt[:], prm[:, :, 2], AF.Exp, R, R)
                self.tt(adt[:], ar, dt[:], M, R, R)
                self.tt(th[:], ai, dt[:], M, R, R)
                b3 = lambda a: a.unsqueeze(2).to_broadcast([128, 8, 32])
                evb = ev[:].unsqueeze(1).to_broadcast([128, 8, 32])
                self.tt(angE[:], b3(th[:]), evb, M, R, R)
                self.tt(magE[:], b3(adt[:]), evb, M, R, R)
                self.act(magE[:], magE[:], AF.Exp, R, R)
                self.sin_of(cosE[:], angE[:], math.pi / 2, kfE[:], kiE[:], Rp)
                self.sin_of(sinE[:], angE[:], 0.0, kfE[:], kiE[:], Rp)
                self.tt(Pr[:], magE[:], cosE[:], M, R, R)
                self.tt(Pi[:], magE[:], sinE[:], M, R, R)
                self.act(rho[:], adt[:], AF.Exp, R, R, scale=8.0)
                self.tsc(t8[:], th[:], 8.0, None, M, None, R, R)
                self.tsc(k8[:], t8[:], 1.0 / (2 * math.pi), None, M, None, R, R)
                self.cp(k8i[:], k8[:], R, R)
                self.cp(k8[:], k8i[:], R, R)
                self.stt(phi[:], k8[:], -6.28125, t8[:], M, A_, R, R)
                self.stt(phi[:], k8[:], -(2 * math.pi - 6.28125), phi[:], M, A_, R, R)
                lr, li = Pr[:, :, 9], Pi[:, :, 9]
                self.tsc(lm1[:], lr, -1.0, None, A_, None, R, R)
                self.tt(den[:], ar, ar, M, R, R)
                self.tt(ta[:], ai, ai, M, R, R)
                self.tt(den[:], den[:], ta[:], A_, R, R)
                self.recip(den[:], den[:], R, R)
                self.tt(ta[:], lm1[:], ar, M, R, R)
                self.tt(tb_[:], li, ai, M, R, R)
                self.tt(zr[:], ta[:], tb_[:], A_, R, R)
                self.tt(zr[:], zr[:], den[:], M, R, R)
                self.tt(ta[:], li, ar, M, R, R)
                self.tt(tb_[:], lm1[:], ai, M, R, R)
                self.tt(zi[:], ta[:], tb_[:], SUB, R, R)
                self.tt(zi[:], zi[:], den[:], M, R, R)
                v16 = lambda n: self.sb(s2, [128, 8, 16], F32, n)[0]
                Bbr, Bbi, u1, u2 = [v16(n) for n in "Bbr Bbi u1 u2".split()]
                Br, Bi = prm[:, :, 3:19], prm[:, :, 19:35]
                Cr, Ci = prm[:, :, 35:51], prm[:, :, 51:67]
                b16 = lambda a: a.unsqueeze(2).to_broadcast([128, 8, 16])
                self.tt(u1[:], b16(zr[:]), Br, M, R, R)
                self.tt(u2[:], b16(zi[:]), Bi, M, R, R)
                self.tt(Bbr[:], u1[:], u2[:], SUB, R, R)
                self.tt(u1[:], b16(zr[:]), Bi, M, R, R)
                self.tt(u2[:], b16(zi[:]), Br, M, R, R)
                self.tt(Bbi[:], u1[:], u2[:], A_, R, R)
                w4 = lambda n: self.sb(s2, [128, 8, 8, 16], F32, n)[0]
                X1r, X1i, X2r, X2i, w1, w2 = [w4(n) for n in "X1r X1i X2r X2i w1 w2".split()]

                def cprod(k0, Xr, Xi, outr, outi, neg_i=False):
                    pk = lambda P_: P_[:, :, k0:k0 + 8].unsqueeze(3).to_broadcast([128, 8, 8, 16])
                    xb = lambda X: X.unsqueeze(2).to_broadcast([128, 8, 8, 16])
                    self.tt(w1[:], pk(Pr), xb(Xr), M, R, R)
                    self.tt(w2[:], pk(Pi), xb(Xi), M, R, R)
                    self.tt(outr[:], w1[:], w2[:], SUB, R, R)
                    self.tt(w1[:], pk(Pr), xb(Xi), M, R, R)
                    self.tt(w2[:], pk(Pi), xb(Xr), M, R, R)
                    if neg_i:
                        self.stt(outi[:], w1[:], -1.0, w2[:], M, SUB, R, R)
                    else:
                        self.tt(outi[:], w1[:], w2[:], A_, R, R)
                fl = lambda X, rows, g8: X[rows, g8].rearrange("p a b -> p (a b)")
                cprod(0, Bbr[:], Bbi[:], X1r, X1i, neg_i=True)
                cprod(8, Cr, Ci, X2r, X2i)
                for g4 in range(4):
                    pt, pr = self.psum()
                    for j in range(4):
                        g = g4 * 4 + j
                        rows, g8 = slice((g % 2) * 64, (g % 2) * 64 + 64), g // 2
                        self.mm(pt[:, j * 128:(j + 1) * 128], fl(X1r, rows, g8), fl(X2r, rows, g8), True, False, R, [pr])
                        self.mm(pt[:, j * 128:(j + 1) * 128], fl(X1i, rows, g8), fl(X2i, rows, g8), False, True, R, [pr])
                    self.tt(Tg[:, g4 * 4:(g4 + 1) * 4, :], pt[:, :].rearrange("p (a b) -> p a b", b=128),
                            msk[:].rearrange("p (a b) -> p a b", b=128), M, [pr], [RTg])
                S.barrier()
                cprod(16, Cr, Ci, X2r, X2i, neg_i=True)
                self.cp(Rpr[:], X2r[:].rearrange("p g a b -> p g (a b)"), R, [RRp])
                self.cp(Rpi[:], X2i[:].rearrange("p g a b -> p g (a b)"), R, [RRp])
                cprod(24, Bbr[:], Bbi[:], X1r, X1i)
                for half in range(2):
                    for ri, X in enumerate((X1r, X1i)):
                        pt, pr = self.psum()
                        for j in range(8):
                            g = half * 8 + j
                            rows, g8 = slice((g % 2) * 64, (g % 2) * 64 + 64), g // 2
                            self.tr(pt[:, j * 64:(j + 1) * 64], fl(X, rows, g8), self.c_ident_f[rows, rows], R, [pr])
                        self.cp(G[:, half * 8:(half + 1) * 8, ri, :], pt[:, :].rearrange("p (a b) -> p a b", b=64), [pr], [RG])
                S.barrier()
            UC, RUC = self.sb(st, [128, 2, 16, 8, 16], BF16, "UC")
            UG, RUG = self.sb(st, [128, 16, 256], BF16, "UG")
            Sre, RS = self.sb(st, [128, 8, 257], BF16, "Sre")
            Sim, _ = self.sb(st, [128, 8, 257], BF16, "Sim")
            wdst = ExitStack()
            wd, Rwd = self.load_w(wdst, dram["W_D"][l], [128, 8, 256], "wd")
            k = 0
            for cb in range(2):
                for i2 in range(4):
                    pt, pr = self.psum()
                    for ii in range(2):
                        i = i2 * 2 + ii
                        for kt in range(8):
                            lhs = hT[:, kt, cb * 1024:(cb + 1) * 1024].rearrange("p (c i) -> p c i", i=8)[:, :, i]
                            self.mm(pt[:, ii * 256:(ii + 1) * 256], lhs, wd[:, kt, :], kt == 0, kt == 7, [Rwd, Rh[cb * 2], Rh[cb * 2 + 1]], [pr])
                    dst = UC[:, cb, :, i2 * 2:(i2 + 1) * 2, :]
                    src = pt[:, :].rearrange("p (ii g h) -> p g ii h", ii=2, g=16, h=16)
                    if k % 2 == 0:
                        self.act(dst, src, AF.Copy, [pr], [RUC])
                    else:
                        self.cp(dst, src, [pr], [RUC])
                    k += 1
            S.barrier()
            wdst.close()
            for cb in range(2):
                for half in range(2):
                    pt, pr = self.psum()
                    ptb = pt[:, :].bitcast(BF16)
                    for j in range(8):
                        g = half * 8 + j
                        self.tr(ptb[:, j * 128:(j + 1) * 128], UC[:, cb, g].rearrange("p i h -> p (i h)"), self.c_ident[:], [RUC], [pr])
                    self.cp(UG[:, half * 8:(half + 1) * 8, cb * 128:(cb + 1) * 128], ptb.rearrange("p (a b) -> p a b", b=128), [pr], [RUG])
            self.memset(Sre[:, :, 0:1], 0.0, [RS])
            self.memset(Sim[:, :, 0:1], 0.0, [RS])
            with ExitStack() as s2:
                cidx, _ = self.sb(s2, [128, 256], F32, "cidx")
                cmask, _ = self.sb(s2, [128, 256], F32, "cmask")
                S.dma("sp", cidx[:], dram["s5_cidx"], writes=[Res("cidx")])
                S.dma("sp", cmask[:], dram["s5_cmask"], writes=[Res("cmask")])
                S.barrier()
                q2 = lambda n, dt_=F32: self.sb(s2, [128, 2, 256], dt_, n)[0]
                Ec, Es, rmask, Zr, Zi, t1, t2, t3, t4 = [q2(n) for n in "Ec Es rmask Zr Zi t1 t2 t3 t4".split()]
                RZ = Res("s5z")
                RZl = [RZ]
                for qd in range(4):
                    gs = slice(qd * 2, qd * 2 + 2)
                    bq = lambda a: a.unsqueeze(2).to_broadcast([128, 2, 256])
                    cb_ = lambda a: a.unsqueeze(1).to_broadcast([128, 2, 256])
                    self.tt(t1[:], bq(phi[:, gs]), cb_(cidx[:]), M, [Rp, RZ], RZl)
                    self.sin_of(Ec[:], t1[:], math.pi / 2, t2[:], t3[:].bitcast(I32), RZ)
                    self.sin_of(Es[:], t1[:], 0.0, t2[:], t3[:].bitcast(I32), RZ)
                    self.tt(rmask[:], bq(rho[:, gs]), cb_(cmask[:]), M, [Rp, RZ], RZl)
                    for gg in range(2):
                        g8 = qd * 2 + gg
                        pz, rpz = self.psum()
                        for g2 in range(2):
                            g = g8 * 2 + g2
                            rows = slice(g2 * 64, g2 * 64 + 64)
                            self.mm(pz[rows, 0:256], G[:, g, 0, :], UG[:, g, :], True, True, [RG, RUG], [rpz])
                            self.mm(pz[rows, 256:512], G[:, g, 1, :], UG[:, g, :], True, True, [RG, RUG], [rpz])
                        self.act(Zr[:, gg, :], pz[:, 0:256], AF.Copy, [rpz], RZl)
                        self.act(Zi[:, gg, :], pz[:, 256:512], AF.Copy, [rpz], RZl)
                    self.tt(t1[:], Ec[:], Zr[:], M, RZl, RZl)
                    self.tt(t2[:], Es[:], Zi[:], M, RZl, RZl)
                    self.tt(t3[:], Ec[:], Zi[:], M, RZl, RZl)
                    self.tt(t4[:], Es[:], Zr[:], M, RZl, RZl)
                    self.tt(Zr[:], t1[:], t2[:], A_, RZl, RZl)
                    self.tt(Zi[:], t3[:], t4[:], SUB, RZl, RZl)
                    fz = lambda a: a[:].rearrange("p a b -> p (a b)")
                    self.scan(fz(Zr), fz(rmask), fz(Zr), 0.0, M, A_, RZl, RZl)
                    self.scan(fz(Zi), fz(rmask), fz(Zi), 0.0, M, A_, RZl, RZl)
                    self.tt(t1[:], Ec[:], Zr[:], M, RZl, RZl)
                    self.tt(t2[:], Es[:], Zi[:], M, RZl, RZl)
                    self.tt(t3[:], Ec[:], Zi[:], M, RZl, RZl)
                    self.tt(t4[:], Es[:], Zr[:], M, RZl, RZl)
                    self.tt(Sre[:, gs, 1:257], t1[:], t2[:], SUB, RZl, [RS])
                    self.tt(Sim[:, gs, 1:257], t3[:], t4[:], A_, RZl, [RS])
                S.barrier()
            with ExitStack() as s2:
                Dt, RD = self.sb(s2, [128, 256], F32, "Dt")
                S.dma("sp", Dt[:], dram["S5D"][l], writes=[RD])
                wglu, Rwglu = self.load_w(s2, dram["W_GLU"][l], [128, 2, 256], "wglu")
                DU, RDU = self.sb(s2, [128, 16, 8, 16], F32, "DU")
                Yf, RYf = self.sb(s2, [128, 16, 8, 16], F32, "Yf")
                Yb, RYb = self.sb(s2, [128, 8, 16, 16], BF16, "Yb")
                yT, _ = self.sb(s2, [128, 2, T], BF16, "yT")
                RyT = [Res("yT0"), Res("yT1")]
                sg, Rsg = self.sb(s2, [128, 512], F32, "sgl")
                for cb in range(2):
                    self.tt(DU[:], UC[:, cb], Dt[:].rearrange("p (g h) -> p g h", h=16).unsqueeze(2).to_broadcast([128, 16, 8, 16]), M,
                            [RUC, RD], [RDU])
                    for g4 in range(4):
                        pt, pr = self.psum()
                        for j in range(4):
                            g = g4 * 4 + j
                            rows, g8 = slice((g % 2) * 64, (g % 2) * 64 + 64), g // 2
                            cs_ = slice(cb * 128, (cb + 1) * 128)
                            o_ = pt[:, j * 128:(j + 1) * 128]
                            self.mm(o_, UG[:, g, cs_], Tg[:, g, :], True, False, [RUG, RTg], [pr])
                            self.mm(o_, Sre[rows, g8, cs_], Rpr[rows, g8, :], False, False, [RS, RRp], [pr])
                            self.mm(o_, Sim[rows, g8, cs_], Rpi[rows, g8, :], False, True, [RS, RRp], [pr])
                        gsl = slice(g4 * 4, (g4 + 1) * 4)
                        self.tt(Yf[:, gsl], pt[:, :].rearrange("c (g j h) -> c g j h", j=8, h=16), DU[:, gsl], A_, [pr, RDU], [RYf])
                    self.act(Yb[:].rearrange("c j g h -> c g j h"), Yf[:], AF.Gelu_apprx_tanh, [RYf], [RYb])
                    for half in range(2):
                        pt, pr = self.psum()
                        ptb = pt[:, :].bitcast(BF16)
                        for j in range(8):
                            self.tr(ptb[:, j * 128:(j + 1) * 128], Yb[:, j].rearrange("c g h -> c (g h)")[:, half * 128:(half + 1) * 128], self.c_ident[:], [RYb], [pr])
                        dst = yT[:, half, cb * 1024:(cb + 1) * 1024].rearrange("p (c j) -> p j c", j=8)
                        self.cp(dst, ptb.rearrange("p (j c) -> p j c", c=128), [pr], [RyT[cb]])
                self.tap("yT", yT[:], RyT[1], [128, 2, T], BF16)
                for tb in range(4):
                    ts = slice(tb * 512, (tb + 1) * 512)
                    for mt in range(2):
                        pt, pr = self.psum()
                        for kt in range(2):
                            self.mm(pt[:, :], wglu[:, kt, mt * 128:(mt + 1) * 128], yT[:, kt, ts], kt == 0, kt == 1, [Rwglu, RyT[tb // 2]], [pr])
                        self.act(sg[:], pt[:, :], AF.Sigmoid, [pr], [Rsg], bias=self.sm(l, "b_glu", mt))
                        self.tt(OUT[:, 3, mt, ts], yT[:, mt, ts], sg[:], M, [RyT[tb // 2], Rsg], [ROUT[3][tb]])
                S.barrier()
            S.barrier()


DRAM_SHAPES = {
    "xT": ([128, 8, T], F32), "memT": ([128, 8, MEM], F32), "pos": ([1, T], I32),
    "ident": ([128, 128], F32), "ones": ([128, 128], F32), "ones_bd": ([128, 128], F32), "negm": ([128, 128], F32),
    "gla_mask": ([128, 512], F32), "ropec": ([128, 2], F32), "reset64": ([128, T], F32), "s5_evec": ([128, 32], F32), "s5_cidx": ([128, 256], F32),
    "s5_mask4": ([128, 512], F32), "s5_cmask": ([128, 256], F32),
}


def build_program(weights, consts, stages=("mix", "xa", "ffn"), nlayers=NL, taps=(), branches="abcdm"):
    nc = bass.Bass("TRN2", target_bir_lowering=False)
    dram = {}
    for k, (shape, dt) in DRAM_SHAPES.items():
        dram[k] = nc.dram_tensor(k, shape, dt, kind="ExternalInput").ap()
    for k, v in weights.items():
        dram[k] = nc.dram_tensor(k, list(v.shape), F32, kind="ExternalInput").ap()
    dram["outT"] = nc.dram_tensor("outT", [128, 8, T], F32, kind="ExternalOutput").ap()
    with ExitStack() as es:
        S = Sched(nc, es)
        kb = KB(nc, es, S, dram, taps)
        kb.branches = branches
        kb.setup(es)
        for l in range(nlayers):
            if "mix" in stages:
                kb.mixer(l)
            if "xa" in stages:
                kb.xattn(l)
            if "ffn" in stages:
                kb.ffn(l)
        kb.store_out()
        S.emit()
    return nc


_CACHE = {}


def kernel(**inputs):
    inp = {k: np.asarray(v) for k, v in inputs.items()}
    weights = prep_weights(inp)
    consts = make_consts()
    nc = build_program(weights, consts)
    in_maps = []
    for b in range(8):
        m = {}
        m["xT"] = np.ascontiguousarray(inp["x"][b].T.reshape(8, 128, T).transpose(1, 0, 2))
        m["memT"] = np.ascontiguousarray(inp["mem"][b].T.reshape(8, 128, MEM).transpose(1, 0, 2))
        m["pos"] = np.ascontiguousarray(inp["positions"][b].reshape(1, T).astype(np.int32))
        m.update(consts)
        m.update(weights)
        in_maps.append(m)
    res = run_bass_kernel_spmd(nc, in_maps, core_ids=list(range(8)))
    out = np.empty((8, T, D), np.float32)
    for b in range(8):
        o = res.results[b]["outT"]
        out[b] = o.transpose(2, 1, 0).reshape(T, D)
    return out
```

```python
import math
import numpy as np
from contextlib import ExitStack
import concourse.bass as bass
import concourse.mybir as mybir
from concourse.bass_utils import run_bass_kernel_spmd

F32 = mybir.dt.float32
BF16 = mybir.dt.bfloat16
I32 = mybir.dt.int32
AF = mybir.ActivationFunctionType
ALU = mybir.AluOpType
AX = mybir.AxisListType

T = 2048
D = 1024
NL = 4
MEM = 256
EPS = 1e-6
ENGS = ("pe", "act", "dve", "pool", "sp")


class Res:
    __slots__ = ("name", "w", "r", "sem", "pe_rows")

    def __init__(self, name=""):
        self.name = name
        self.w = None
        self.r = {}
        self.sem = None
        self.pe_rows = None


class Sched:
    def __init__(self, nc, es):
        self.nc = nc
        self.es = es
        self.q = {e: [] for e in ENGS}
        self.cnt = {}
        self.known = {e: {} for e in ENGS}
        self.sems = {}
        self.free_dma_sems = []
        self.dma_res = []
        for e in ENGS:
            self._sem("eng_" + e)
        self.n_dma_sems = 0

    def _sem(self, key):
        if key not in self.sems:
            self.sems[key] = self.es.enter_context(self.nc.semaphore(key))
            self.cnt[key] = 0
        return self.sems[key]

    def _deps(self, eng, reads, writes, pe_inorder=False):
        need = {}

        def add(s, n):
            if need.get(s, 0) < n:
                need[s] = n
        for r in reads:
            if r.w is not None:
                add(*r.w)
        for w in writes:
            if w.w is not None:
                add(*w.w)
            for s, n in w.r.items():
                add(s, n)
        waits = []
        kn = self.known[eng]
        if eng == "pe" and pe_inorder:
            need.pop("eng_pe", None)
        for s, n in need.items():
            if kn.get(s, 0) < n:
                waits.append((s, n))
                kn[s] = n
        return waits

    def _commit(self, ticket, reads, writes):
        s, n = ticket
        for r in reads:
            if r.r.get(s, 0) < n:
                r.r[s] = n
        for w in writes:
            w.w = ticket
            w.r = {}

    def op(self, eng, fn, reads=(), writes=(), pe_inorder=False):
        waits = self._deps(eng, reads, writes, pe_inorder)
        key = "eng_" + eng
        self.cnt[key] += 1
        ticket = (key, self.cnt[key])
        self.q[eng].append((waits, fn, (key, 1)))
        self._commit(ticket, reads, writes)
        return ticket

    def dma(self, eng, out, in_, reads=(), writes=(), **kw):
        tgt = writes[0] if writes else reads[0]
        if tgt.sem is None:
            if self.free_dma_sems:
                tgt.sem = self.free_dma_sems.pop()
            else:
                tgt.sem = "dma_%d" % self.n_dma_sems
                self.n_dma_sems += 1
            self.dma_res.append(tgt)
        semkey = tgt.sem
        self._sem(semkey)
        waits = self._deps(eng, reads, writes)
        self.cnt[semkey] += 16
        ticket = (semkey, self.cnt[semkey])
        self.q[eng].append((waits, lambda e: e.dma_start(out=out, in_=in_, **kw), (semkey, 16)))
        self._commit(ticket, reads, writes)
        return ticket

    def barrier(self, recycle=()):
        snap = [(k, self.cnt[k]) for k in self.sems if self.cnt[k] > 0]
        for e in ENGS:
            kn = self.known[e]
            waits = [(s, n) for s, n in snap if kn.get(s, 0) < n]
            for s, n in waits:
                kn[s] = n
            if waits:
                self.q[e].append((waits, None, None))
        for r in self.dma_res:
            if r.sem is not None:
                if r.sem not in self.free_dma_sems:
                    self.free_dma_sems.append(r.sem)
                r.sem = None
        self.dma_res = []

    def emit(self):
        nc = self.nc
        with nc.Block() as block:
            def replay(e, name):
                for waits, fn, inc in self.q[name]:
                    for s, n in waits:
                        e.wait_ge(self.sems[s], n)
                    if fn is None:
                        continue
                    ins = fn(e)
                    ins.then_inc(self.sems[inc[0]], inc[1])

            @block.tensor
            def _(e):
                replay(e, "pe")

            @block.scalar
            def _(e):
                replay(e, "act")

            @block.vector
            def _(e):
                replay(e, "dve")

            @block.gpsimd
            def _(e):
                replay(e, "pool")

            @block.sync
            def _(e):
                replay(e, "sp")


def _kmaj(w):
    K, N = w.shape
    return np.ascontiguousarray(w.reshape(K // 128, 128, N).transpose(1, 0, 2))


def _pp(v):
    return np.ascontiguousarray(v.reshape(-1, 128).T)


SMALL_COLS = {}
_off = 0
for _n, _w in [("g_mix_pre", 8), ("g_mix_post", 8), ("g_xa_pre", 8), ("g_xa_post", 8), ("g_mem", 8),
               ("g_ffn_pre", 8), ("g_ffn_post", 8), ("q_norm", 2), ("kv_norm", 1), ("hg_out_norm", 2),
               ("hg_lb_logits", 8), ("conv_w", 8), ("conv_b", 2), ("i_bias", 2), ("f_bias", 2),
               ("ml_out_norm", 2), ("b_glu", 2)]:
    SMALL_COLS[_n] = (_off, _w)
    _off += _w
NSMALL = _off


def prep_layer_small(inp, l):
    s = np.zeros((128, NSMALL), np.float32)

    def put(name, arr):
        o, w = SMALL_COLS[name]
        assert arr.shape == (128, w), (name, arr.shape)
        s[:, o:o + w] = arr
    for n, k in [("g_mix_pre", "norm_mix_pre"), ("g_mix_post", "norm_mix_post"), ("g_xa_pre", "norm_xa_pre"),
                 ("g_xa_post", "norm_xa_post"), ("g_mem", "norm_mem"), ("g_ffn_pre", "norm_ffn_pre"),
                 ("g_ffn_post", "norm_ffn_post")]:
        put(n, _pp(inp[k][l]))
    put("q_norm", _pp(inp["mla_q_norm"][l]))
    put("kv_norm", _pp(inp["mla_kv_norm"][l]))
    put("hg_out_norm", _pp(inp["hg_out_norm"][l]))
    lg = inp["hg_lb_logits"]
    put("hg_lb_logits", np.ascontiguousarray(lg.T.reshape(2, 128, NL).transpose(1, 0, 2).reshape(128, 8)))
    cw = inp["ml_conv_w"][l]
    put("conv_w", np.ascontiguousarray(cw.reshape(4, 2, 128).transpose(2, 0, 1).reshape(128, 8)))
    put("conv_b", _pp(inp["ml_conv_b"][l]))
    put("i_bias", _pp(np.repeat(inp["ml_i_bias"][l], 64)))
    put("f_bias", _pp(np.repeat(inp["ml_f_bias"][l], 64)))
    put("ml_out_norm", _pp(inp["ml_out_norm"][l]))
    put("b_glu", _pp(inp["s5_b_glu"][l]))
    return s


_SPL = np.cumsum([0, 256, 128, 32, 256, 256, 256, 256, 256, 256, 256, 4, 4, 256, 4096])
(C_AQ, C_AKV, C_AKR, C_BQ, C_BF, C_BI, C_BG, C_CX, C_CV, C_CO, C_CIG, C_CFG, C_DU, C_GATE, _) = [int(v) for v in _SPL]


def prep_weights(inp):
    W = {}
    w_in = inp["w_in"]
    perm32 = np.concatenate([np.arange(16, 32), np.arange(0, 16)])
    wa, wuq, wukv, wb, wc, wd, wg = [], [], [], [], [], [], []
    wbr, wout, xq, xk, xv, xo, f1, f2, mlq, mlk, wglu = [], [], [], [], [], [], [], [], [], [], []
    small = []
    s5p, s5d = [], []
    for l in range(NL):
        wi = w_in[l]
        akr = wi[:, C_AKR:C_AKR + 32]
        a = np.concatenate([wi[:, C_AQ:C_AQ + 256], wi[:, C_AKV:C_AKV + 128], akr, akr[:, perm32]], axis=1)
        wa.append(_kmaj(a))
        uq = inp["mla_w_uq"][l]
        uqp = np.concatenate([uq[:, h * 96 + 64:h * 96 + 96][:, perm32] for h in range(4)], axis=1)
        wuq.append(_kmaj(np.concatenate([uq, uqp], axis=1)))
        wukv.append(np.ascontiguousarray(inp["mla_w_ukv"][l]))
        wbk = _kmaj(wi[:, C_BQ:C_BQ + 1024]).reshape(128, 8, 4, 2, 128)
        wb.append(np.ascontiguousarray(wbk.transpose(3, 0, 1, 2, 4)))
        ig = np.repeat(wi[:, C_CIG:C_CIG + 4], 64, axis=1)
        fg = np.repeat(wi[:, C_CFG:C_CFG + 4], 64, axis=1)
        wck = _kmaj(np.concatenate([wi[:, C_CX:C_CX + 768], ig, fg], axis=1)).reshape(128, 8, 5, 2, 128)
        wc.append(np.ascontiguousarray(wck.transpose(3, 0, 1, 2, 4)))
        wd.append(_kmaj(wi[:, C_DU:C_DU + 256]))
        g = wi[:, C_GATE:C_GATE + 4096].reshape(8, 128, 4, 8, 128)
        wg.append(np.ascontiguousarray(g.transpose(3, 1, 2, 0, 4)))
        br = inp["w_branch"][l].reshape(4, 2, 128, 1024)
        wbr.append(np.ascontiguousarray(br.transpose(2, 0, 1, 3)))
        wout.append(_kmaj(inp["w_out"][l]))
        xq.append(_kmaj(inp["xa_wq"][l]))
        xk.append(_kmaj(inp["xa_wk"][l]))
        xv.append(_kmaj(inp["xa_wv"][l]))
        xo.append(_kmaj(inp["xa_wo"][l]))
        fi = inp["ffn_w_in"][l]
        pieces = [np.concatenate([fi[:, j * 128:(j + 1) * 128], fi[:, 2816 + j * 128:2816 + (j + 1) * 128]], axis=1)
                  for j in range(22)]
        f1.append(np.stack([_kmaj(p) for p in pieces]))
        fo = inp["ffn_w_out"][l].reshape(22, 128, 8, 128)
        f2.append(np.ascontiguousarray(fo.transpose(2, 1, 0, 3)))
        q_ = inp["ml_w_q"][l].reshape(2, 2, 64, 64)
        k_ = inp["ml_w_k"][l].reshape(2, 2, 64, 64)
        mlq.append(np.ascontiguousarray(q_.transpose(1, 2, 0, 3).reshape(128, 2, 64)))
        mlk.append(np.ascontiguousarray(k_.transpose(1, 2, 0, 3).reshape(128, 2, 64)))
        wglu.append(_kmaj(inp["s5_w_glu"][l]))
        def pl(a):
            sh = a.shape[2:]
            return np.ascontiguousarray(a.reshape(8, 2, 64, *sh).transpose(1, 2, 0, *range(3, 3 + len(sh))).reshape(128, 8, *sh))
        sp = np.zeros((128, 8, 67), np.float32)
        sp[:, :, 0] = pl(inp["s5_a_re"][l])
        sp[:, :, 1] = pl(inp["s5_a_im"][l])
        sp[:, :, 2] = pl(np.repeat(inp["s5_log_dt"][l][:, None], 64, axis=1))
        sp[:, :, 3:19] = pl(inp["s5_b_re"][l])
        sp[:, :, 19:35] = pl(inp["s5_b_im"][l])
        sp[:, :, 35:51] = pl(inp["s5_c_re"][l].transpose(0, 2, 1))
        sp[:, :, 51:67] = pl(inp["s5_c_im"][l].transpose(0, 2, 1))
        s5p.append(sp)
        s5d.append(np.repeat(inp["s5_d"][l].reshape(1, 256), 128, axis=0))
        small.append(prep_layer_small(inp, l))
    W["W_A"] = np.stack(wa)
    W["W_UQ"] = np.stack(wuq)
    W["W_UKV"] = np.stack(wukv)
    W["W_B"] = np.stack(wb)
    W["W_C"] = np.stack(wc)
    W["W_D"] = np.stack(wd)
    W["W_G"] = np.stack(wg)
    W["W_BR"] = np.stack(wbr)
    W["W_OUT"] = np.stack(wout)
    W["XA_Q"] = np.stack(xq)
    W["XA_K"] = np.stack(xk)
    W["XA_V"] = np.stack(xv)
    W["XA_O"] = np.stack(xo)
    W["W_F1"] = np.stack(f1)
    W["W_F2"] = np.stack(f2)
    W["ML_Q"] = np.stack(mlq)
    W["ML_K"] = np.stack(mlk)
    W["W_GLU"] = np.stack(wglu)
    W["SMALL"] = np.stack(small)
    W["S5P"] = np.stack(s5p)
    W["S5D"] = np.stack(s5d)
    return {k: np.ascontiguousarray(v, dtype=np.float32) for k, v in W.items()}


def make_consts():
    import ml_dtypes
    C = {}
    C["ident"] = np.eye(128, dtype=np.float32)
    C["ones"] = np.ones((128, 128), np.float32)
    bd = np.zeros((128, 128), np.float32)
    bd[:64, :64] = 1
    bd[64:, 64:] = 1
    C["ones_bd"] = bd
    s = np.arange(128)[:, None]
    t = np.arange(128)[None, :]
    C["negm"] = np.where(s <= t, 0.0, -30000.0).astype(np.float32)
    gm = ((s // 64 == t // 64) & (s <= t)).astype(np.float32)
    C["gla_mask"] = np.tile(gm, (1, 4))
    inv_freq = (10000.0 ** (-np.arange(16, dtype=np.float32) / 16)).astype(np.float32)
    ropec = np.zeros((128, 2), np.float32)
    ropec[64:96, 0] = np.concatenate([inv_freq, inv_freq])
    ropec[64:80, 1] = -1.0
    ropec[80:96, 1] = 1.0
    C["ropec"] = ropec
    rm = np.ones((128, T), np.float32)
    rm[:, ::64] = 0.0
    C["reset64"] = rm
    ev = np.concatenate([-np.arange(8), np.arange(8), np.arange(8) + 1, 7 - np.arange(8)]).astype(np.float32)
    C["s5_evec"] = np.tile(ev[None, :], (128, 1))
    C["s5_cidx"] = np.tile(np.arange(256, dtype=np.float32)[None, :], (128, 1))
    kk = np.arange(128)
    C["s5_mask4"] = np.tile(((kk[None, :] // 16) >= (kk[:, None] // 16)).astype(np.float32), (1, 4))
    cm = np.ones((128, 256), np.float32)
    cm[:, 0] = 0.0
    C["s5_cmask"] = cm
    return C


class KB:
    def __init__(self, nc, es, S, dram, taps):
        self.nc, self.es, self.S, self.dram = nc, es, S, dram
        self.taps = taps
        self.uid = 0
        self.ps = []
        for i in range(8):
            t = es.enter_context(nc.psum_tensor("ps%d" % i, [128, 512], F32))
            self.ps.append((t, Res("ps%d" % i)))
        self.ps_i = 0
        self.held = set()

    def psum(self):
        while True:
            i = self.ps_i
            self.ps_i = (self.ps_i + 1) % 8
            if i not in self.held:
                return self.ps[i]

    def psum_hold(self):
        while True:
            i = self.ps_i
            self.ps_i = (self.ps_i + 1) % 8
            if i not in self.held:
                self.held.add(i)
                return self.ps[i] + (i,)

    def psum_release(self, i):
        self.held.discard(i)

    def sb(self, stack, shape, dtype, name=None):
        self.uid += 1
        nm = "%s_%d" % (name or "t", self.uid)
        t = stack.enter_context(self.nc.sbuf_tensor(nm, list(shape), dtype))
        return t, Res(nm)

    def tap(self, name, ap, res, shape, dtype=F32):
        if name not in self.taps:
            return
        d = self.nc.dram_tensor("tap_" + name, list(shape), dtype, kind="ExternalOutput").ap()
        self.S.dma("sp", d, ap, reads=[res], writes=[Res("tap_" + name)])

    def mm(self, out, lhsT, rhs, start, stop, reads, writes):
        b0 = lhsT.base_partition()
        rows = (b0, b0 + lhsT.shape[0])
        w = writes[0]
        prev = w.pe_rows
        inorder = prev is None or not (rows[1] <= prev[0] or prev[1] <= rows[0])
        w.pe_rows = rows
        self.S.op("pe", lambda e: e.matmul(out, lhsT=lhsT, rhs=rhs, start=start, stop=stop), reads, writes, pe_inorder=inorder)

    def tr(self, out, in_, ident, reads, writes):
        self.S.op("pe", lambda e: e.transpose(out, in_, ident), reads, writes)

    def act(self, out, in_, func, reads, writes, **kw):
        self.S.op("act", lambda e: e.activation(out=out, in_=in_, func=func, **kw), reads, writes)

    def tt(self, out, in0, in1, op, reads, writes, eng="dve"):
        self.S.op(eng, lambda e: e.tensor_tensor(out=out, in0=in0, in1=in1, op=op), reads, writes)

    def tsc(self, out, in0, s1, s2, op0, op1, reads, writes, eng="dve"):
        if s2 is None:
            self.S.op(eng, lambda e: e.tensor_scalar(out=out, in0=in0, scalar1=s1, scalar2=None, op0=op0), reads, writes)
        else:
            self.S.op(eng, lambda e: e.tensor_scalar(out=out, in0=in0, scalar1=s1, scalar2=s2, op0=op0, op1=op1), reads, writes)

    def stt(self, out, in0, scalar, in1, op0, op1, reads, writes, eng="dve"):
        self.S.op(eng, lambda e: e.scalar_tensor_tensor(out=out, in0=in0, scalar=scalar, in1=in1, op0=op0, op1=op1), reads, writes)

    def cp(self, out, in_, reads, writes, eng="dve"):
        self.S.op(eng, lambda e: e.tensor_copy(out=out, in_=in_), reads, writes)

    def scan(self, out, d0, d1, init, op0, op1, reads, writes):
        self.S.op("dve", lambda e: e.tensor_tensor_scan(out=out, data0=d0, data1=d1, initial=init, op0=op0, op1=op1), reads, writes)

    def memset(self, ap, val, writes, eng="dve"):
        self.S.op(eng, lambda e: e.memset(ap, val), (), writes)

    def recip(self, out, in_, reads, writes):
        self.act(out, in_, AF.Ln, reads, writes)
        self.act(out, out, AF.Exp, writes, writes, scale=-1.0)

    def load_w(self, stack, dram_ap, shape, name="w", eng="pool"):
        t, r = self.sb(stack, shape, BF16, name)
        self.S.dma(eng, t[:], dram_ap, writes=[r])
        return t, r

    def rstd(self, out, sum_ps, nfeat, reads, Rout, nrows=128):
        self.act(out, sum_ps, AF.Ln, reads, [Rout], scale=1.0 / nfeat, bias=self.c_eps[0:nrows, :])
        self.act(out, out, AF.Exp, [Rout], [Rout], scale=-0.5)

    def setup(self, stack, pre_x=None):
        S, dram = self.S, self.dram
        self.c_ident_f, _ = self.sb(stack, [128, 128], F32, "identf")
        self.c_ident, _ = self.sb(stack, [128, 128], BF16, "ident")
        self.c_ones, _ = self.sb(stack, [128, 128], BF16, "ones")
        self.c_ones_bd, _ = self.sb(stack, [128, 128], BF16, "onesbd")
        self.c_negm, _ = self.sb(stack, [128, 128], BF16, "negm")
        self.c_gla, _ = self.sb(stack, [128, 512], BF16, "glam")
        self.c_ropec, _ = self.sb(stack, [128, 2], F32, "ropec")
        self.c_eps, _ = self.sb(stack, [128, 1], F32, "eps")
        self.c_rm, _ = self.sb(stack, [128, 512], F32, "rm64")
        self.c_one, _ = self.sb(stack, [128, 1], F32, "one")
        self.small, _ = self.sb(stack, [128, NL, NSMALL], F32, "small")
        for dst, src, eng in [(self.c_ident_f, "ident", "sp"), (self.c_ident, "ident", "pool"),
                              (self.c_ones, "ones", "pool"), (self.c_ones_bd, "ones_bd", "pool"),
                              (self.c_negm, "negm", "pool"), (self.c_gla, "gla_mask", "pool"),
                              (self.c_ropec, "ropec", "sp")]:
            S.dma(eng, dst[:], dram[src], writes=[Res("c_" + src)])
        S.dma("act", self.c_rm[:], dram["reset64"][:, 0:512], writes=[Res("c_rm")])
        S.dma("sp", self.small[:], dram["SMALL"].rearrange("l p n -> p l n"), writes=[Res("c_small")])
        self.memset(self.c_eps[:], EPS, [Res("c_eps")])
        self.memset(self.c_one[:], 1.0, [Res("c_one")])
        if pre_x is not None:
            S.barrier()
            self.Rc = Res("consts")
            pre_x()
        self.xT, _ = self.sb(stack, [128, 8, T], F32, "xT")
        self.Rxb = [Res("x_tb%d" % i) for i in range(4)]
        for tb in range(4):
            S.dma("sp" if tb % 2 == 0 else "act", self.xT[:, :, tb * 512:(tb + 1) * 512],
                  dram["xT"][:, :, tb * 512:(tb + 1) * 512], writes=[self.Rxb[tb]])
        S.barrier()
        self.Rc = Res("consts")

    def sm(self, l, name, c=None, rows=slice(0, 128)):
        o, w = SMALL_COLS[name]
        if c is None:
            return self.small[rows, l, o:o + w]
        return self.small[rows, l, o + c:o + c + 1]

    def norm_fm(self, stack, src, src_rs, dst, dst_rs, l, gname, nK, nfeat, ntb=4, ncol=512):
        with ExitStack() as st:
            sq, _ = self.sb(st, [128, 2, ncol], BF16, "sq")
            Rsq2 = [Res("sq0"), Res("sq1")]
            rstd, Rrstd = self.sb(st, [128, ncol], F32, "rstd")
            tmp, Rtmp = self.sb(st, [128, ncol], F32, "tmp")
            for tb in range(ntb):
                ts = slice(tb * ncol, (tb + 1) * ncol)
                pt, pr = self.psum()
                for kt in range(nK):
                    self.act(sq[:, kt % 2, :], src[:, kt, ts], AF.Square, [src_rs[tb]], [Rsq2[kt % 2]])
                    self.mm(pt[:, 0:ncol], self.c_ones[:], sq[:, kt % 2, :], kt == 0, kt == nK - 1, [Rsq2[kt % 2]], [pr])
                self.rstd(rstd[:], pt[:, 0:ncol], nfeat, [pr], Rrstd)
                for kt in range(nK):
                    self.stt(dst[:, kt, ts], src[:, kt, ts], self.sm(l, gname, kt), rstd[:], ALU.mult, ALU.mult,
                             [src_rs[tb], Rrstd], [dst_rs[tb]])
            self.S.barrier()

    def postnorm_block(self, l, gname, y, Ry, tb, scr):
        sq, Rsq2, rstd, Rrstd, tmp, Rtmp = scr
        ts = slice(tb * 512, (tb + 1) * 512)
        pt, pr = self.psum()
        for m in range(8):
            self.act(sq[:, m % 2, :], y[:, m, :], AF.Square, [Ry], [Rsq2[m % 2]])
            self.mm(pt[:, :], self.c_ones[:], sq[:, m % 2, :], m == 0, m == 7, [Rsq2[m % 2]], [pr])
        self.rstd(rstd[:], pt[:, :], D, [pr], Rrstd)
        for m in range(8):
            self.tt(tmp[:], y[:, m, :], rstd[:], ALU.mult, [Ry, Rrstd], [Rtmp])
            self.stt(self.xT[:, m, ts], tmp[:], self.sm(l, gname, m), self.xT[:, m, ts], ALU.mult, ALU.add,
                     [Rtmp, self.Rxb[tb]], [self.Rxb[tb]])

    def postnorm_scratch(self, stack):
        sq, _ = self.sb(stack, [128, 2, 512], BF16, "psq")
        Rsq = [Res("psq0"), Res("psq1")]
        rstd, Rrstd = self.sb(stack, [128, 512], F32, "prstd")
        tmp, Rtmp = self.sb(stack, [128, 512], F32, "ptmp")
        return (sq, Rsq, rstd, Rrstd, tmp, Rtmp)

    def store_out(self):
        for tb in range(4):
            self.S.dma("sp" if tb % 2 == 0 else "act", self.dram["outT"][:, :, tb * 512:(tb + 1) * 512],
                       self.xT[:, :, tb * 512:(tb + 1) * 512], reads=[self.Rxb[tb]], writes=[Res("o%d" % tb)])
        self.S.barrier()

    def wslots(self, stack, shape, n, name="ws"):
        slots = [self.sb(stack, shape, BF16, name) for _ in range(n)]
        state = {"i": 0}

        def load(dram_ap, sub=None):
            t, r = slots[state["i"] % n]
            state["i"] += 1
            dst = t[:] if sub is None else sub(t)
            self.S.dma("pool", dst, dram_ap, writes=[r])
            return t, r
        return load

    def ffn(self, l):
        S, dram = self.S, self.dram
        with ExitStack() as st:
            hid, _ = self.sb(st, [128, 22, T], BF16, "hid")
            Rhid = [Res("hid%d" % i) for i in range(4)]
            with ExitStack() as st2:
                hT, _ = self.sb(st2, [128, 8, T], BF16, "hT")
                Rh = [Res("h%d" % i) for i in range(4)]
                self.norm_fm(st2, self.xT, self.Rxb, hT, Rh, l, "g_ffn_pre", 8, D)
                sil, Rsil = self.sb(st2, [128, 2, 512], F32, "sil")
                load1 = self.wslots(st2, [128, 8, 256], 2, "wf1")
                for j in range(22):
                    w, Rw = load1(dram["W_F1"][l, j])
                    for tb in range(4):
                        ts = slice(tb * 512, (tb + 1) * 512)
                        pa, ra = self.psum()
                        pb, rb = self.psum()
                        for kt in range(8):
                            self.mm(pa[:, :], w[:, kt, 0:128], hT[:, kt, ts], kt == 0, kt == 7, [Rw, Rh[tb]], [ra])
                        for kt in range(8):
                            self.mm(pb[:, :], w[:, kt, 128:256], hT[:, kt, ts], kt == 0, kt == 7, [Rw, Rh[tb]], [rb])
                        self.act(sil[:, tb % 2, :], pa[:, :], AF.Silu, [ra], [Rsil])
                        self.tt(hid[:, j, ts], sil[:, tb % 2, :], pb[:, :], ALU.mult, [Rsil, rb], [Rhid[tb]])
                S.barrier()
            with ExitStack() as st2:
                y2 = [self.sb(st2, [128, 8, 512], F32, "y") for _ in range(2)]
                scr = self.postnorm_scratch(st2)
                load2 = self.wslots(st2, [128, 22, 128], 2, "wf2")
                for half in range(2):
                    for m in range(8):
                        w, Rw = load2(dram["W_F2"][l, m])
                        for t2 in range(2):
                            tb = half * 2 + t2
                            ts = slice(tb * 512, (tb + 1) * 512)
                            y, Ry = y2[t2]
                            pt, pr = self.psum()
                            for kt in range(22):
                                self.mm(pt[:, :], w[:, kt, :], hid[:, kt, ts], kt == 0, kt == 21, [Rw, Rhid[tb]], [pr])
                            self.act(y[:, m, :], pt[:, :], AF.Copy, [pr], [Ry])
                    for t2 in range(2):
                        self.postnorm_block(l, "g_ffn_post", y2[t2][0], y2[t2][1], half * 2 + t2, scr)
                S.barrier()

    def xattn(self, l):
        S, dram = self.S, self.dram
        with ExitStack() as st:
            QT, _ = self.sb(st, [128, 8, T], BF16, "QT")
            RQ = [Res("q%d" % i) for i in range(4)]
            KT, RK = self.sb(st, [128, 8, MEM], BF16, "KT")
            V, RV = self.sb(st, [128, 2, D], BF16, "V")
            load = self.wslots(st, [128, 8, D], 2, "wxa")
            with ExitStack() as st2:
                memf, Rmemf = self.sb(st2, [128, 8, MEM], F32, "memf")
                memn, Rmemn = self.sb(st2, [128, 8, MEM], BF16, "memn")
                S.dma("sp", memf[:], dram["memT"], writes=[Rmemf])
                self.norm_fm(st2, memf, [Rmemf], memn, [Rmemn], l, "g_mem", 8, D, ntb=1, ncol=MEM)
                wk, Rwk = load(dram["XA_K"][l])
                for c in range(8):
                    pt, pr = self.psum()
                    for kt in range(8):
                        self.mm(pt[:, 0:MEM], wk[:, kt, c * 128:(c + 1) * 128], memn[:, kt, :], kt == 0, kt == 7, [Rwk, Rmemn], [pr])
                    self.act(KT[:, c, :], pt[:, 0:MEM], AF.Copy, [pr], [RK])
                wv, Rwv = load(dram["XA_V"][l])
                for mt in range(2):
                    for nb in range(2):
                        pt, pr = self.psum()
                        for kt in range(8):
                            self.mm(pt[:, :], memn[:, kt, mt * 128:(mt + 1) * 128], wv[:, kt, nb * 512:(nb + 1) * 512],
                                    kt == 0, kt == 7, [Rwv, Rmemn], [pr])
                        self.cp(V[:, mt, nb * 512:(nb + 1) * 512], pt[:, :], [pr], [RV])
                hT, _ = self.sb(st2, [128, 8, T], BF16, "hT")
                Rh = [Res("h%d" % i) for i in range(4)]
                self.norm_fm(st2, self.xT, self.Rxb, hT, Rh, l, "g_xa_pre", 8, D)
                wq, Rwq = load(dram["XA_Q"][l])
                for c in range(8):
                    for tb in range(4):
                        ts = slice(tb * 512, (tb + 1) * 512)
                        pt, pr = self.psum()
                        for kt in range(8):
                            self.mm(pt[:, :], wq[:, kt, c * 128:(c + 1) * 128], hT[:, kt, ts], kt == 0, kt == 7, [Rwq, Rh[tb]], [pr])
                        if (c + tb) % 2 == 0:
                            self.act(QT[:, c, ts], pt[:, :], AF.Copy, [pr], [RQ[tb]])
                        else:
                            self.cp(QT[:, c, ts], pt[:, :], [pr], [RQ[tb]])
                S.barrier()
            with ExitStack() as st2:
                AO, _ = self.sb(st2, [128, 8, T], BF16, "AO")
                RAO = [Res("ao%d" % i) for i in range(4)]
                P, _ = self.sb(st2, [128, 2, 2, 512], BF16, "P")
                RP = [Res("P0"), Res("P1")]
                rinv, Rrinv = self.sb(st2, [128, 512], F32, "rinv")
                wo, Rwo = load(dram["XA_O"][l])
                its = [(tb, h) for tb in range(4) for h in range(4)]

                def xscore(it):
                    tb, h = its[it]
                    ts = slice(tb * 512, (tb + 1) * 512)
                    for mt in range(2):
                        pt, pr = self.psum()
                        for dt in range(2):
                            self.mm(pt[:, :], KT[:, h * 2 + dt, mt * 128:(mt + 1) * 128], QT[:, h * 2 + dt, ts],
                                    dt == 0, dt == 1, [RK, RQ[tb]], [pr])
                        self.act(P[:, it % 2, mt, :], pt[:, :], AF.Exp, [pr], [RP[it % 2]], scale=1.0 / 16.0)

                xscore(0)
                for it, (tb, h) in enumerate(its):
                    ts = slice(tb * 512, (tb + 1) * 512)
                    if it + 1 < len(its):
                        xscore(it + 1)
                    Pi, RPi = P[:, it % 2], RP[it % 2]
                    pt, pr = self.psum()
                    for mt in range(2):
                        self.mm(pt[:, :], self.c_ones[:], Pi[:, mt, :], mt == 0, mt == 1, [RPi], [pr])
                    self.recip(rinv[:], pt[:, :], [pr], [Rrinv])
                    for et in range(2):
                        pt, pr = self.psum()
                        for mt in range(2):
                            self.mm(pt[:, :], V[:, mt, h * 256 + et * 128:h * 256 + (et + 1) * 128], Pi[:, mt, :],
                                    mt == 0, mt == 1, [RV, RPi], [pr])
                        self.tt(AO[:, h * 2 + et, ts], pt[:, :], rinv[:], ALU.mult, [pr, Rrinv], [RAO[tb]])
                ya, Rya = self.sb(st2, [128, 8, 512], F32, "y")
                yb = wq[:].bitcast(F32)
                y2 = [(ya, Rya), (yb, Rwq)]
                scr = self.postnorm_scratch(st2)
                def oproj(tb):
                    ts = slice(tb * 512, (tb + 1) * 512)
                    y, Ry = y2[tb % 2]
                    for m in range(8):
                        pt, pr = self.psum()
                        for kt in range(8):
                            self.mm(pt[:, :], wo[:, kt, m * 128:(m + 1) * 128], AO[:, kt, ts], kt == 0, kt == 7, [Rwo, RAO[tb]], [pr])
                        self.act(y[:, m, :], pt[:, :], AF.Copy, [pr], [Ry])

                oproj(0)
                for tb in range(4):
                    if tb + 1 < 4:
                        oproj(tb + 1)
                    self.postnorm_block(l, "g_xa_post", y2[tb % 2][0], y2[tb % 2][1], tb, scr)
                S.barrier()

    def rope_tables(self, stack):
        S = self.S
        cs, Rcs = self.sb(stack, [128, 2, T], F32, "cs")
        with ExitStack() as st:
            posi, Rpi = self.sb(st, [128, T], I32, "posi")
            ang, Ra = self.sb(st, [128, T], F32, "ang")
            kf, Rk = self.sb(st, [128, T], F32, "kf")
            ki, Rki = posi, Rpi
            rows = slice(64, 96)
            S.dma("sp", posi[rows, :], self.dram["pos"].partition_broadcast(32), writes=[Rpi])
            self.cp(ang[rows, :], posi[rows, :], [Rpi], [Ra])
            self.tsc(ang[rows, :], ang[rows, :], self.c_ropec[rows, 0:1], None, ALU.mult, None, [Ra], [Ra])
            for which in range(2):
                shift = math.pi / 2 if which == 0 else 0.0
                self.tsc(kf[rows, :], ang[rows, :], shift, 1.0 / (2 * math.pi), ALU.add, ALU.mult, [Ra], [Rk])
                self.cp(ki[rows, :], kf[rows, :], [Rk], [Rki])
                self.cp(kf[rows, :], ki[rows, :], [Rki], [Rk])
                c1 = 6.28125
                c2 = 2 * math.pi - c1
                dst = cs[rows, which, :]
                self.stt(dst, kf[rows, :], -c1, ang[rows, :], ALU.mult, ALU.add, [Rk, Ra], [Rcs])
                self.stt(dst, kf[rows, :], -c2, dst, ALU.mult, ALU.add, [Rk, Rcs], [Rcs])
                self.tsc(dst, dst, shift, 3.1415925, ALU.add, ALU.min, [Rcs], [Rcs])
                self.tsc(dst, dst, -3.1415925, None, ALU.max, None, [Rcs], [Rcs])
                self.act(dst, dst, AF.Sin, [Rcs], [Rcs])
            self.tsc(cs[rows, 1, :], cs[rows, 1, :], self.c_ropec[rows, 1:2], None, ALU.mult, None, [Rcs], [Rcs])
            S.barrier()
        return cs, Rcs

    def mla(self, l, hT, Rh, OUT, ROUT):
        S, dram = self.S, self.dram
        scale = 96 ** -0.5
        with ExitStack() as s1:
            wa, Rwa = self.load_w(s1, dram["W_A"][l], [128, 8, 448], "wa")
            wuq, Rwuq = self.load_w(s1, dram["W_UQ"][l], [128, 2, 512], "wuq")
            wukv, Rwukv = self.load_w(s1, dram["W_UKV"][l], [128, 512], "wukv")
            aqn, _ = self.sb(s1, [128, 2, T], BF16, "aqn")
            akvn, _ = self.sb(s1, [128, T], BF16, "akvn")
            krope, _ = self.sb(s1, [128, T], BF16, "krope")
            Raqn = [Res("aqn%d" % i) for i in range(4)]
            Rakvn = [Res("akvn%d" % i) for i in range(4)]
            Rkr = [Res("kr%d" % i) for i in range(4)]
            cs, Rcs = self.rope_tables(s1)
            rr = slice(64, 96)
            with ExitStack() as s2:
                af, Raf = self.sb(s2, [128, 3, 512], F32, "af")
                sq, Rsq = self.sb(s2, [128, 3, 512], BF16, "sq")
                rs, Rrs = self.sb(s2, [128, 2, 512], F32, "rs")
                t1, Rt1 = self.sb(s2, [128, 2, 512], F32, "t1")
                for tb in range(4):
                    ts = slice(tb * 512, (tb + 1) * 512)
                    for mt in range(3):
                        pt, pr = self.psum()
                        for kt in range(8):
                            self.mm(pt[:, :], wa[:, kt, mt * 128:(mt + 1) * 128], hT[:, kt, ts], kt == 0, kt == 7, [Rwa, Rh[tb]], [pr])
                        self.act(af[:, mt, :], pt[:, :], AF.Copy, [pr], [Raf])
                        self.act(sq[:, mt, :], pt[:, :], AF.Square, [pr], [Rsq])
                    pq, prq = self.psum()
                    self.mm(pq[:, :], self.c_ones[:], sq[:, 0, :], True, False, [Rsq], [prq])
                    self.mm(pq[:, :], self.c_ones[:], sq[:, 1, :], False, True, [Rsq], [prq])
                    self.rstd(rs[:, 0, :], pq[:, :], 256, [prq], Rrs)
                    pk, prk = self.psum()
                    self.mm(pk[:, :], self.c_ones[:], sq[:, 2, :], True, True, [Rsq], [prk])
                    self.rstd(rs[:, 1, :], pk[:, :], 128, [prk], Rrs)
                    for mt in range(2):
                        self.tt(t1[:, 0, :], af[:, mt, :], rs[:, 0, :], ALU.mult, [Raf, Rrs], [Rt1])
                        self.tsc(aqn[:, mt, ts], t1[:, 0, :], self.sm(l, "q_norm", mt), None, ALU.mult, None, [Rt1], [Raqn[tb]])
                    self.tt(t1[:, 1, :], af[:, 2, :], rs[:, 1, :], ALU.mult, [Raf, Rrs], [Rt1])
                    self.tsc(akvn[:, ts], t1[:, 1, :], self.sm(l, "kv_norm", 0), None, ALU.mult, None, [Rt1], [Rakvn[tb]])
                    pa, pra = self.psum()
                    pb, prb = self.psum()
                    for kt in range(8):
                        self.mm(pa[rr, :], wa[:, kt, 384:416], hT[:, kt, ts], kt == 0, kt == 7, [Rwa, Rh[tb]], [pra])
                    for kt in range(8):
                        self.mm(pb[rr, :], wa[:, kt, 416:448], hT[:, kt, ts], kt == 0, kt == 7, [Rwa, Rh[tb]], [prb])
                    self.tt(t1[rr, 0, :], pa[rr, :], cs[rr, 0, ts], ALU.mult, [pra, Rcs], [Rt1])
                    self.tt(t1[rr, 1, :], pb[rr, :], cs[rr, 1, ts], ALU.mult, [prb, Rcs], [Rt1])
                    self.tt(krope[rr, ts], t1[rr, 0, :], t1[rr, 1, :], ALU.add, [Rt1], [Rkr[tb]])
                S.barrier()
            for h in range(4):
                with ExitStack() as s2:
                    QT, _ = self.sb(s2, [128, T], BF16, "QT")
                    KT, _ = self.sb(s2, [128, T], BF16, "KT")
                    VP, RVP = self.sb(s2, [128, 16, 128], BF16, "VP")
                    RQ = [Res("Q%d" % i) for i in range(4)]
                    RK = [Res("K%d" % i) for i in range(4)]
                    t1, Rt1 = self.sb(s2, [128, 2, 512], F32, "t1")
                    rden, Rrden = self.sb(s2, [128, 512], F32, "rden")
                    odd = h % 2
                    vcol = slice(64, 128) if odd else slice(0, 64)
                    ocol = slice(0, 64) if odd else slice(64, 128)
                    self.memset(VP[:, :, ocol], 1.0, [RVP])
                    for tb in range(4):
                        ts = slice(tb * 512, (tb + 1) * 512)
                        qa, rqa = self.psum()
                        qb, rqb = self.psum()
                        for kt in range(2):
                            self.mm(qa[0:96, :], wuq[:, kt, h * 96:(h + 1) * 96], aqn[:, kt, ts], kt == 0, kt == 1, [Rwuq, Raqn[tb]], [rqa])
                        for kt in range(2):
                            self.mm(qb[rr, :], wuq[:, kt, 384 + h * 32:384 + (h + 1) * 32], aqn[:, kt, ts], kt == 0, kt == 1,
                                    [Rwuq, Raqn[tb]], [rqb])
                        self.act(QT[0:64, ts], qa[0:64, :], AF.Copy, [rqa], [RQ[tb]])
                        self.tt(t1[rr, 0, :], qa[rr, :], cs[rr, 0, ts], ALU.mult, [rqa, Rcs], [Rt1])
                        self.tt(t1[rr, 1, :], qb[rr, :], cs[rr, 1, ts], ALU.mult, [rqb, Rcs], [Rt1])
                        self.tt(QT[rr, ts], t1[rr, 0, :], t1[rr, 1, :], ALU.add, [Rt1], [RQ[tb]])
                        kn, rkn = self.psum()
                        self.mm(kn[0:64, :], wukv[:, h * 128:h * 128 + 64], akvn[:, ts], True, True, [Rwukv, Rakvn[tb]], [rkn])
                        self.act(KT[0:64, ts], kn[0:64, :], AF.Copy, [rkn], [RK[tb]])
                        self.act(KT[rr, ts], krope[rr, ts], AF.Copy, [Rkr[tb]], [RK[tb]])
                    for g8 in range(2):
                        pv, rpv = self.psum()
                        for j in range(8):
                            tt_ = g8 * 8 + j
                            self.mm(pv[:, j * 64:(j + 1) * 64], akvn[:, tt_ * 128:(tt_ + 1) * 128], wukv[:, h * 128 + 64:h * 128 + 128],
                                    True, True, [Rwukv, Rakvn[tt_ // 4]], [rpv])
                        self.cp(VP[:, g8 * 8:(g8 + 1) * 8, vcol], pv[:, :].rearrange("p (a b) -> p a b", b=64), [rpv], [RVP])
                    Pt4, _ = self.sb(s2, [128, 4, 512], BF16, "Pt4")
                    RP4 = [Res("P4_%d" % i) for i in range(4)]
                    for tb in range(4):
                        ts = slice(tb * 512, (tb + 1) * 512)
                        po, rpo, hold = self.psum_hold()
                        nst = 4 * tb + 4

                        def score(s_):
                            t0 = max(0, s_ * 128 - tb * 512)
                            N = 512 - t0
                            ps, rps = self.psum()
                            diag = s_ * 128 >= tb * 512
                            self.mm(ps[:, 0:N], KT[0:96, s_ * 128:(s_ + 1) * 128], QT[0:96, tb * 512 + t0:(tb + 1) * 512], True, not diag,
                                    [RK[s_ // 4], RQ[tb]], [rps])
                            if diag:
                                self.mm(ps[:, 0:128], self.c_ident[:], self.c_negm[:], False, True, [], [rps])
                            self.act(Pt4[:, s_ % 4, 0:N], ps[:, 0:N], AF.Exp, [rps], [RP4[s_ % 4]], scale=scale)

                        score(0)
                        for s_ in range(nst):
                            if s_ + 1 < nst:
                                score(s_ + 1)
                            t0 = max(0, s_ * 128 - tb * 512)
                            N = 512 - t0
                            self.mm(po[:, t0:512], VP[:, s_, :], Pt4[:, s_ % 4, 0:N], s_ == 0, s_ == nst - 1, [RVP, RP4[s_ % 4]], [rpo])
                        hs = slice(64, 128) if odd else slice(0, 64)
                        ds = slice(0, 64) if odd else slice(64, 128)
                        self.recip(rden[hs, :], po[ds, :], [rpo], [Rrden])
                        self.tt(OUT[hs, 0, h // 2, ts], po[hs, :], rden[hs, :], ALU.mult, [rpo, Rrden], [ROUT[0][tb]])
                        self.psum_release(hold)
                    S.barrier()
            S.barrier()

    def merge(self, l, hT, Rh, OUT, ROUT):
        S, dram = self.S, self.dram
        with ExitStack() as s1:
            merged, _ = self.sb(s1, [128, 8, T], BF16, "merged")
            Rmg = [Res("mg%d" % i) for i in range(4)]
            with ExitStack() as s2:
                wbr, Rwbr = self.load_w(s2, dram["W_BR"][l], [128, 4, 2, D], "wbr")
                load = self.wslots(s2, [128, 8, 128], 3, "wg")
                sig, Rsig = self.sb(s2, [128, 2, 512], F32, "sig")
                tmp, Rtmp = self.sb(s2, [128, 512], F32, "tmpm")
                k = 0
                for m in range(8):
                    for n in range(4):
                        wg, Rwg = load(dram["W_G"][l, m, :, n])
                        for tb in range(4):
                            ts = slice(tb * 512, (tb + 1) * 512)
                            pg, rpg = self.psum()
                            for kt in range(8):
                                self.mm(pg[:, :], wg[:, kt, :], hT[:, kt, ts], kt == 0, kt == 7, [Rwg, Rh[tb]], [rpg])
                            pb, rpb = self.psum()
                            for kt in range(2):
                                self.mm(pb[:, :], wbr[:, n, kt, m * 128:(m + 1) * 128], OUT[:, n, kt, ts], kt == 0, kt == 1,
                                        [Rwbr, ROUT[n][tb]], [rpb])
                            sg = sig[:, k % 2, :]
                            k += 1
                            self.act(sg, pg[:, :], AF.Sigmoid, [rpg], [Rsig])
                            if n == 0:
                                self.tt(self.macc[:, tb, :], sg, pb[:, :], ALU.mult, [Rsig, rpb], [self.Rmacc[tb]])
                            else:
                                self.tt(tmp[:], sg, pb[:, :], ALU.mult, [Rsig, rpb], [Rtmp])
                                if n < 3:
                                    self.tt(self.macc[:, tb, :], self.macc[:, tb, :], tmp[:], ALU.add, [Rtmp, self.Rmacc[tb]], [self.Rmacc[tb]])
                                else:
                                    self.tt(merged[:, m, ts], self.macc[:, tb, :], tmp[:], ALU.add, [Rtmp, self.Rmacc[tb]], [Rmg[tb]])
                S.barrier()
            S.barrier()
            wout = hT[:, 0:4, :].rearrange("p a (b c) -> p (a b) c", c=D)
            y = hT[:, 4:8, :].bitcast(F32).rearrange("p a (b c) -> p (a b) c", c=512)
            Rwout, Ry = Res("wout"), Res("y")
            S.dma("pool", wout, dram["W_OUT"][l], writes=[Rwout])
            o_sq = OUT[:, 0, 0, :].rearrange("p (a b) -> p a b", b=512)[:, 0:2, :]
            o_f = OUT[:, 1, :, :].bitcast(F32)
            scr = (o_sq, [Res("psq0"), Res("psq1")], o_f[:, 0, 0:512], Res("prstd"), o_f[:, 0, 512:1024], Res("ptmp"))
            yb = OUT[:, 2:4, :, :].bitcast(F32).rearrange("p a b (c d) -> p (a b c) d", d=512)
            y2 = [(y, Ry), (yb, Res("yb"))]
            def oproj(tb):
                ts = slice(tb * 512, (tb + 1) * 512)
                yy, Ryy = y2[tb % 2]
                for m in range(8):
                    pt, pr = self.psum()
                    for kt in range(8):
                        self.mm(pt[:, :], wout[:, kt, m * 128:(m + 1) * 128], merged[:, kt, ts], kt == 0, kt == 7, [Rwout, Rmg[tb]], [pr])
                    self.act(yy[:, m, :], pt[:, :], AF.Copy, [pr], [Ryy])

            oproj(0)
            for tb in range(4):
                if tb + 1 < 4:
                    oproj(tb + 1)
                self.postnorm_block(l, "g_mix_post", y2[tb % 2][0], y2[tb % 2][1], tb, scr)
            S.barrier()

    def mixer(self, l):
        with ExitStack() as st:
            hT, _ = self.sb(st, [128, 8, T], BF16, "hT")
            Rh = [Res("h%d" % i) for i in range(4)]
            OUT, _ = self.sb(st, [128, 4, 2, T], BF16, "OUT")
            ROUT = [[Res("out%d_%d" % (n, i)) for i in range(4)] for n in range(4)]
            self.norm_fm(st, self.xT, self.Rxb, hT, Rh, l, "g_mix_pre", 8, D)
            self.tap("hT", hT[:], Rh[3], [128, 8, T], BF16)
            if "a" in self.branches:
                self.mla(l, hT, Rh, OUT, ROUT)
            if "b" in self.branches:
                self.hgrn2(l, hT, Rh, OUT, ROUT)
            if "c" in self.branches:
                self.mlstm(l, hT, Rh, OUT, ROUT)
            if "d" in self.branches:
                self.s5(l, hT, Rh, OUT, ROUT)
            for n, nm in enumerate("abcd"):
                if nm not in self.branches:
                    for tb in range(4):
                        self.memset(OUT[:, n, :, tb * 512:(tb + 1) * 512], 0.0, [ROUT[n][tb]], eng="pool")
            self.tap("OUT", OUT[:], ROUT[0][3], [128, 4, 2, T], BF16)
            if "m" in self.branches:
                with ExitStack() as s2:
                    self.macc, _ = self.sb(s2, [128, 4, 512], F32, "macc")
                    self.Rmacc = [Res("macc%d" % i) for i in range(4)]
                    self.merge(l, hT, Rh, OUT, ROUT)
            self.S.barrier()

    def gla_pair(self, st, prep_alloc, prep_tb, VPp, RVP, EP, post_alloc, post, out_base_by_hh):
        S = self.S
        QK, _ = self.sb(st, [128, 4, T], BF16, "QK")
        qh, qt, kt_, kh = QK[:, 0, :], QK[:, 1, :], QK[:, 2, :], QK[:, 3, :]
        RQK = [Res("QK%d" % i) for i in range(4)]
        Rqh = Rqt = Rkt = Rkh = RQK
        dec, Rdec = self.sb(st, [128, 32], F32, "dec")
        with ExitStack() as s2:
            prep_alloc(s2)
            X, RX = self.sb(s2, [128, 5, 512], F32, "X")
            EX, REX = self.sb(s2, [128, 4, 512], F32, "EX")
            QKin, _ = self.sb(s2, [128, 2, 2, 512], F32, "QKin")
            RQKin = [Res("QKin0"), Res("QKin1")]
            for tb in range(4):
                ts = slice(tb * 512, (tb + 1) * 512)
                qk, Rqk = QKin[:, tb % 2], RQKin[tb % 2]
                lf, Rlf, ig, Rig = prep_tb(tb, qk[:, 0, :], qk[:, 1, :], Rqk)
                b = X[:, 0, :]
                self.scan(b, self.c_rm[:], lf, 0.0, ALU.mult, ALU.add, [Rlf], [RX])
                bch = b.rearrange("p (c j) -> p c j", j=64)
                ref = bch[:, :, 31:64:32].rearrange("p c r -> p r c").unsqueeze(3).to_broadcast([128, 2, 8, 64])
                self.tt(X[:, 1:3, :].rearrange("p r (c j) -> p r c j", j=64), bch.unsqueeze(1).to_broadcast([128, 2, 8, 64]), ref,
                        ALU.subtract, [RX], [RX])
                self.act(dec[:, tb * 8:(tb + 1) * 8], bch[:, :, 63], AF.Exp, [RX], [Rdec])
                if ig is not None:
                    self.tt(X[:, 3:5, :], ig.unsqueeze(1).to_broadcast([128, 2, 512]), X[:, 1:3, :], ALU.subtract, [Rig, RX], [RX])
                else:
                    self.tsc(X[:, 3:5, :], X[:, 1:3, :], -1.0, None, ALU.mult, None, [RX], [RX])
                self.act(EX[:, 0:2, :], X[:, 0:2, :], AF.Exp, [RX], [REX])
                self.act(EX[:, 2:4, :], X[:, 3:5, :], AF.Exp, [RX], [REX])
                self.tt(QK[:, :, ts].rearrange("p (a r) t -> p a r t", r=2), qk.unsqueeze(2).to_broadcast([128, 2, 2, 512]),
                        EX[:].rearrange("p (a r) t -> p a r t", r=2), ALU.mult, [Rqk, REX], [RQK[tb]])
            S.barrier()
        khT, RkhT = self.sb(st, [128, 16, 128], BF16, "khT")
        snap, _ = self.sb(st, [128, 32, EP], BF16, "snap")
        Rsnap = [Res("snap%d" % i) for i in range(32)]
        Sf2, _ = self.sb(st, [128, 2, EP], F32, "Sf")
        RSf2 = [Res("Sf0"), Res("Sf1")]
        Abf, _ = self.sb(st, [128, 4, 512], BF16, "Abf")
        RA = [Res("A%d" % i) for i in range(4)]
        post_alloc(st)
        for g8 in range(2):
            pt, pr = self.psum()
            ptb = pt[:, :].bitcast(BF16)
            for j in range(8):
                blk = g8 * 8 + j
                self.tr(ptb[:, j * 128:(j + 1) * 128], kh[:, blk * 128:(blk + 1) * 128], self.c_ident[:], [Rkh[blk // 4]], [pr])
            self.cp(khT[:, g8 * 8:(g8 + 1) * 8, :], ptb.rearrange("p (a b) -> p a b", b=128), [pr], [RkhT])
        self.memset(Sf2[:, 0, :], 0.0, [RSf2[0]])
        self.memset(snap[:, 0, :], 0.0, [Rsnap[0]])
        for c in range(31):
            blk, base = c // 2, (c % 2) * 64
            pk, rpk = self.psum()
            for hh in range(2):
                self.mm(pk[hh * 64:(hh + 1) * 64, 0:EP], khT[base:base + 64, blk, hh * 64:(hh + 1) * 64],
                        VPp[base:base + 64, blk, hh, :], True, True, [RkhT, RVP], [rpk])
            so, sn = Sf2[:, c % 2, :], Sf2[:, (c + 1) % 2, :]
            self.stt(sn, so, dec[:, c:c + 1], pk[:, 0:EP], ALU.mult, ALU.add, [RSf2[c % 2], Rdec, rpk], [RSf2[(c + 1) % 2]])
            self.act(snap[:, c + 1, :], sn, AF.Copy, [RSf2[(c + 1) % 2]], [Rsnap[c + 1]])
        NG = EP // 64

        def scores(tb):
            for hh in range(2):
                hs = slice(hh * 64, (hh + 1) * 64)
                pa, rpa = self.psum()
                for j in range(4):
                    blk = tb * 4 + j
                    bs = slice(blk * 128, (blk + 1) * 128)
                    self.mm(pa[:, j * 128:(j + 1) * 128], kt_[hs, bs], qt[hs, bs], True, True, [Rkt[tb], Rqt[tb]], [rpa])
                k_ = (tb % 2) * 2 + hh
                self.tt(Abf[:, k_, :], pa[:, :], self.c_gla[:], ALU.mult, [rpa], [RA[k_]])

        def values(tb):
            pos = []
            for g in range(NG):
                gc = slice(g * 64, (g + 1) * 64)
                po, rpo, hold = self.psum_hold()
                for hh in range(2):
                    hs = slice(hh * 64, (hh + 1) * 64)
                    k_ = (tb % 2) * 2 + hh
                    A, RAi = Abf[:, k_, :], RA[k_]
                    for j in range(4):
                        blk = tb * 4 + j
                        self.mm(po[hs, j * 128:(j + 1) * 128], VPp[:, blk, hh, gc], A[:, j * 128:(j + 1) * 128], True, False, [RVP, RAi], [rpo])
                        for cc in range(2):
                            c = blk * 2 + cc
                            cs_ = slice(blk * 128 + cc * 64, blk * 128 + (cc + 1) * 64)
                            self.mm(po[hs, j * 128 + cc * 64:j * 128 + (cc + 1) * 64], snap[hs, c, gc], qh[hs, cs_], False, cc == 1,
                                    [Rsnap[c], Rqh[tb]], [rpo])
                pos.append((po, rpo, hold))
            return pos

        scores(0)
        scores(1)
        pend = {0: values(0)}
        for tb in range(4):
            if tb + 2 < 4:
                scores(tb + 2)
            if tb + 1 < 4:
                pend[tb + 1] = values(tb + 1)
            pos = pend.pop(tb)
            post(tb, pos)
            for _, _, hold in pos:
                self.psum_release(hold)

    def headnorm_out(self, src, Rsrc, gain, gate, Rgate, out, Rout, scr):
        sq, Rsq, rs, Rrs, tmp, Rtmp = scr
        self.act(sq[:], src, AF.Square, [Rsrc], [Rsq])
        pt, pr = self.psum()
        self.mm(pt[:, :], self.c_ones_bd[:], sq[:], True, True, [Rsq], [pr])
        self.act(rs[:], pt[:, :], AF.Ln, [pr], [Rrs], scale=1.0 / 64, bias=self.c_eps[:])
        self.act(rs[:], rs[:], AF.Exp, [Rrs], [Rrs], scale=-0.5)
        self.tt(tmp[:], src, rs[:], ALU.mult, [Rsrc, Rrs], [Rtmp])
        self.stt(out, tmp[:], gain, gate, ALU.mult, ALU.mult, [Rtmp, Rgate], [Rout])

    def hgrn2(self, l, hT, Rh, OUT, ROUT):
        S, dram = self.S, self.dram
        with ExitStack() as s0:
            lbt, Rlb = self.sb(s0, [128, 2, 8], F32, "lbt")
            o, w = SMALL_COLS["hg_lb_logits"]
            lg = self.small[:, l, o:o + w].rearrange("p (t j) -> p t j", j=4)
            self.act(lbt[:, :, 0:4], lg, AF.Exp, [], [Rlb])
            for t_ in range(2):
                self.S.op("dve", lambda e, t_=t_: e.reduce_sum(out=lbt[:, t_, 4:5], in_=lbt[:, t_, 0:4], axis=AX.X), [Rlb], [Rlb])
                self.memset(lbt[:, t_, 5:6], 0.0, [Rlb])
                for j in range(1, l + 1):
                    self.tt(lbt[:, t_, 5:6], lbt[:, t_, 5:6], lbt[:, t_, j:j + 1], ALU.add, [Rlb], [Rlb])
                self.recip(lbt[:, t_, 4:5], lbt[:, t_, 4:5], [Rlb], [Rlb])
                self.tt(lbt[:, t_, 5:6], lbt[:, t_, 5:6], lbt[:, t_, 4:5], ALU.mult, [Rlb], [Rlb])
                self.tsc(lbt[:, t_, 6:7], lbt[:, t_, 5:6], -1.0, 1.0, ALU.mult, ALU.add, [Rlb], [Rlb])
                self.tsc(lbt[:, t_, 7:8], lbt[:, t_, 6:7], -1.0, None, ALU.mult, None, [Rlb], [Rlb])
            for pp in range(2):
                with ExitStack() as st:
                    wsrc = dram["W_B"][l, pp]
                    wb, Rwb = self.load_w(st, wsrc, [128, 8, 4, 128], "wb")
                    VPp, RVP = self.sb(st, [128, 16, 2, 64], BF16, "VPp")
                    Tm = {}

                    def prep_alloc(stk):
                        for nm in ("lf", "sg"):
                            Tm[nm] = self.sb(stk, [128, 512], F32, nm)

                    def post_alloc(stk):
                        Tm["gate"] = [self.sb(stk, [128, 512], BF16, "gate") for _ in range(4)]
                        Tm["scr"] = []
                        for _ in range(2):
                            sq, Rsq = self.sb(stk, [128, 512], BF16, "hsq")
                            rs, Rrs = self.sb(stk, [128, 512], F32, "hrs")
                            tmp, Rtmp = self.sb(stk, [128, 512], F32, "htmp")
                            Tm["scr"].append((sq, Rsq, rs, Rrs, tmp, Rtmp))
                        for tb in range(4):
                            gate, Rgate = Tm["gate"][tb]
                            pg, rg = proj(3, tb)
                            self.act(gate[:], pg[:, :], AF.Silu, [rg], [Rgate])
                    for g4 in range(4):
                        pv, rpv = self.psum()
                        for j in range(4):
                            blk = g4 * 4 + j
                            for kt in range(8):
                                self.mm(pv[:, j * 128:(j + 1) * 128], hT[:, kt, blk * 128:(blk + 1) * 128], wb[:, kt, 2, :], kt == 0, kt == 7,
                                        [Rwb, Rh[blk // 4]], [rpv])
                        self.cp(VPp[:, g4 * 4:(g4 + 1) * 4, :, :], pv[:, :].rearrange("p (a h e) -> p a h e", h=2, e=64), [rpv], [RVP])

                    def proj(a, tb):
                        pt, pr = self.psum()
                        for kt in range(8):
                            self.mm(pt[:, :], wb[:, kt, a, :], hT[:, kt, tb * 512:(tb + 1) * 512], kt == 0, kt == 7, [Rwb, Rh[tb]], [pr])
                        return pt, pr

                    def prep_tb(tb, qf, kf, Rqk):
                        (lf, Rlf), (sg, Rsg) = Tm["lf"], Tm["sg"]
                        pq, rq = proj(0, tb)
                        self.act(qf, pq[:, :], AF.Silu, [rq], [Rqk])
                        pf, rf = proj(1, tb)
                        self.act(sg[:], pf[:, :], AF.Sigmoid, [rf], [Rsg])
                        self.tsc(lf[:], sg[:], lbt[:, pp, 6:7], lbt[:, pp, 5:6], ALU.mult, ALU.add, [Rsg, Rlb], [Rlf])
                        self.tsc(lf[:], lf[:], 1e-30, None, ALU.max, None, [Rlf], [Rlf])
                        self.act(lf[:], lf[:], AF.Ln, [Rlf], [Rlf])
                        self.tsc(kf, sg[:], lbt[:, pp, 7:8], lbt[:, pp, 6:7], ALU.mult, ALU.add, [Rsg, Rlb], [Rqk])
                        return lf[:], Rlf, None, None

                    def post(tb, pos):
                        ts = slice(tb * 512, (tb + 1) * 512)
                        gate, Rgate = Tm["gate"][tb]
                        po, rpo, _ = pos[0]
                        self.headnorm_out(po[:, :], rpo, self.sm(l, "hg_out_norm", pp), gate[:], Rgate,
                                          OUT[:, 1, pp, ts], ROUT[1][tb], Tm["scr"][tb % 2])

                    self.gla_pair(st, prep_alloc, prep_tb, VPp, RVP, 64, post_alloc, post, lambda hh: slice(hh * 64, (hh + 1) * 64))
                    S.barrier()
            S.barrier()

    def mlstm(self, l, hT, Rh, OUT, ROUT):
        S, dram = self.S, self.dram
        for pp in range(2):
            with ExitStack() as st:
                wview = dram["W_C"][l, pp]
                mlq, Rmlq = self.load_w(st, dram["ML_Q"][l], [128, 2, 64], "mlq")
                mlk, Rmlk = self.load_w(st, dram["ML_K"][l], [128, 2, 64], "mlk")
                wc, Rwc = self.load_w(st, wview[:, :, 1:5, :], [128, 8, 4, 128], "wc")
                VPp, RVP = self.sb(st, [128, 16, 2, 128], BF16, "VPp")
                xc, _ = self.sb(st, [128, T], BF16, "xc")
                Rxc = [Res("xc%d" % i) for i in range(4)]
                nfb, Rnfb = self.sb(st, [128, 1], F32, "nfb")
                self.tsc(nfb[:], self.sm(l, "f_bias", pp), -1.0, None, ALU.mult, None, [], [Rnfb])
                self.memset(VPp[:, :, :, 64:128], 1.0, [RVP])

                def proj(w_ap, tb, reads):
                    pt, pr = self.psum()
                    for kt in range(8):
                        self.mm(pt[:, :], w_ap(kt), hT[:, kt, tb * 512:(tb + 1) * 512], kt == 0, kt == 7, reads + [Rh[tb]], [pr])
                    return pt, pr
                with ExitStack() as s2:
                    wx, Rwx = self.load_w(s2, wview[:, :, 0, :], [128, 8, 128], "wx")
                    cxf, Rcx = self.sb(s2, [128, 4 + T], F32, "cxf")
                    acc, Racc = self.sb(s2, [128, 512], F32, "acc")
                    self.memset(cxf[:, 0:4], 0.0, [Rcx])
                    for tb in range(4):
                        pt, pr = proj(lambda kt: wx[:, kt, :], tb, [Rwx])
                        self.act(cxf[:, 4 + tb * 512:4 + (tb + 1) * 512], pt[:, :], AF.Copy, [pr], [Rcx])
                    o, _w = SMALL_COLS["conv_w"]
                    for tb in range(4):
                        base = 4 + tb * 512
                        cw = lambda j: self.small[:, l, o + j * 2 + pp:o + j * 2 + pp + 1]
                        self.tsc(acc[:], cxf[:, base:base + 512], cw(3), None, ALU.mult, None, [Rcx], [Racc])
                        for j in range(3):
                            sh = 3 - j
                            self.stt(acc[:], cxf[:, base - sh:base - sh + 512], cw(j), acc[:], ALU.mult, ALU.add, [Rcx, Racc], [Racc])
                        self.act(xc[:, tb * 512:(tb + 1) * 512], acc[:], AF.Silu, [Racc], [Rxc[tb]], bias=self.sm(l, "conv_b", pp))
                    S.barrier()
                for g4 in range(4):
                    pv, rpv = self.psum()
                    for j in range(4):
                        blk = g4 * 4 + j
                        for kt in range(8):
                            self.mm(pv[:, j * 128:(j + 1) * 128], hT[:, kt, blk * 128:(blk + 1) * 128], wc[:, kt, 0, :], kt == 0, kt == 7,
                                    [Rwc, Rh[blk // 4]], [rpv])
                    pvv = pv[:, :].rearrange("p (a h e) -> p a h e", h=2, e=64)
                    self.cp(VPp[:, g4 * 4:(g4 + 1) * 4, :, 0:64], pvv, [rpv], [RVP])
                Tm = {}

                def prep_alloc(stk):
                    for nm in ("lf", "ig", "e1"):
                        Tm[nm] = self.sb(stk, [128, 512], F32, nm)

                def post_alloc(stk):
                    Tm["gate"] = [self.sb(stk, [128, 512], BF16, "gate") for _ in range(4)]
                    Tm["aden"] = [self.sb(stk, [128, 512], F32, "aden") for _ in range(2)]
                    Tm["hml"] = [self.sb(stk, [128, 512], F32, "hml")] * 2
                    Tm["scr"] = []
                    for _ in range(2):
                        sq, Rsq = self.sb(stk, [128, 512], BF16, "hsq")
                        rs, Rrs = self.sb(stk, [128, 512], F32, "hrs")
                        tmp, Rtmp = self.sb(stk, [128, 512], F32, "htmp")
                        Tm["scr"].append((sq, Rsq, rs, Rrs, tmp, Rtmp))
                    for tb in range(4):
                        gate, Rgate = Tm["gate"][tb]
                        pg, rg = proj(lambda kt: wc[:, kt, 1, :], tb, [Rwc])
                        self.act(gate[:], pg[:, :], AF.Sigmoid, [rg], [Rgate])

                def prep_tb(tb, qf, kf, Rqk):
                    (lf, Rlf), (ig, Rig), (e1, Re1) = Tm["lf"], Tm["ig"], Tm["e1"]
                    ts = slice(tb * 512, (tb + 1) * 512)
                    pq, rq = self.psum()
                    pk, rk = self.psum()
                    for hh in range(2):
                        hs = slice(hh * 64, (hh + 1) * 64)
                        self.mm(pq[hs, :], mlq[hs, pp, :], xc[hs, ts], True, True, [Rmlq, Rxc[tb]], [rq])
                        self.mm(pk[hs, :], mlk[hs, pp, :], xc[hs, ts], True, True, [Rmlk, Rxc[tb]], [rk])
                    self.act(qf, pq[:, :], AF.Copy, [rq], [Rqk])
                    self.act(kf, pk[:, :], AF.Copy, [rk], [Rqk], scale=0.125)
                    pf, rf = proj(lambda kt: wc[:, kt, 3, :], tb, [Rwc])
                    self.act(e1[:], pf[:, :], AF.Exp, [rf, Rnfb], [Re1], scale=-1.0, bias=nfb[:])
                    self.act(e1[:], e1[:], AF.Ln, [Re1], [Re1], bias=self.c_one[:])
                    self.tsc(lf[:], e1[:], -1.0, None, ALU.mult, None, [Re1], [Rlf])
                    pi_, ri = proj(lambda kt: wc[:, kt, 2, :], tb, [Rwc])
                    self.act(ig[:], pi_[:, :], AF.Identity, [ri], [Rig], bias=self.sm(l, "i_bias", pp))
                    return lf[:], Rlf, ig[:], Rig

                def post(tb, pos):
                    ts = slice(tb * 512, (tb + 1) * 512)
                    (gate, Rgate), (aden, Raden), (hml, Rhml) = Tm["gate"][tb], Tm["aden"][tb % 2], Tm["hml"][tb % 2]
                    (pn, rpn, _), (pd, rpd, _) = pos
                    self.act(aden[:], pd[:, :], AF.Abs, [rpd], [Raden])
                    self.tsc(aden[:], aden[:], 1.0, None, ALU.max, None, [Raden], [Raden])
                    self.recip(aden[:], aden[:], [Raden], [Raden])
                    self.tt(hml[:], pn[:, :], aden[:], ALU.mult, [rpn, Raden], [Rhml])
                    self.headnorm_out(hml[:], Rhml, self.sm(l, "ml_out_norm", pp), gate[:], Rgate,
                                      OUT[:, 2, pp, ts], ROUT[2][tb], Tm["scr"][tb % 2])

                self.gla_pair(st, prep_alloc, prep_tb, VPp, RVP, 128, post_alloc, post, lambda hh: slice(0, 128))
                S.barrier()
        S.barrier()

    def sin_of(self, dst, ang, shift, kf, ki, R):
        c1 = 6.28125
        c2 = 2 * math.pi - c1
        self.tsc(kf, ang, shift, 1.0 / (2 * math.pi), ALU.add, ALU.mult, [R], [R])
        self.cp(ki, kf, [R], [R])
        self.cp(kf, ki, [R], [R])
        self.stt(dst, kf, -c1, ang, ALU.mult, ALU.add, [R], [R])
        self.stt(dst, kf, -c2, dst, ALU.mult, ALU.add, [R], [R])
        self.tsc(dst, dst, shift, 3.1415925, ALU.add, ALU.min, [R], [R])
        self.tsc(dst, dst, -3.1415925, None, ALU.max, None, [R], [R])
        self.act(dst, dst, AF.Sin, [R], [R])

    def s5_params_all(self):
        S, dram, nc = self.S, self.dram, self.nc
        M, A_, SUB = ALU.mult, ALU.add, ALU.subtract
        NG = NL * 8
        self.s5scr = {
            "Tg": nc.dram_tensor("s5scr_Tg", [128, NL, 16, 128], BF16, kind="ExternalOutput").ap(),
            "G": nc.dram_tensor("s5scr_G", [128, NL, 16, 2, 64], BF16, kind="ExternalOutput").ap(),
            "Rpr": nc.dram_tensor("s5scr_Rpr", [128, NG, 128], BF16, kind="ExternalOutput").ap(),
            "Rpi": nc.dram_tensor("s5scr_Rpi", [128, NG, 128], BF16, kind="ExternalOutput").ap(),
            "rho": nc.dram_tensor("s5scr_rho", [128, NG], F32, kind="ExternalOutput").ap(),
            "phi": nc.dram_tensor("s5scr_phi", [128, NG], F32, kind="ExternalOutput").ap(),
            "Ec": nc.dram_tensor("s5scr_Ec", [128, NG, 256], F32, kind="ExternalOutput").ap(),
            "Es": nc.dram_tensor("s5scr_Es", [128, NG, 256], F32, kind="ExternalOutput").ap(),
        }
        with ExitStack() as s1, ExitStack() as s2:
            Tg, RTg = self.sb(s1, [128, NL, 16, 128], BF16, "Tg")
            G, RG = self.sb(s1, [128, NL, 16, 2, 64], BF16, "G")
            Rpr, RRp = self.sb(s1, [128, NG, 128], BF16, "Rpr")
            Rpi, _ = self.sb(s1, [128, NG, 128], BF16, "Rpi")
            rho, Rp = self.sb(s1, [128, NG], F32, "rho")
            phi, _ = self.sb(s1, [128, NG], F32, "phi")
            prm, _ = self.sb(s2, [128, NG, 67], F32, "prm")
            ev, _ = self.sb(s2, [128, 32], F32, "ev")
            msk, _ = self.sb(s2, [128, 512], F32, "msk")
            S.dma("sp", prm[:].rearrange("p (l g) c -> p l g c", l=NL), dram["S5P"].rearrange("l p g c -> p l g c"), writes=[Res("prm")])
            S.dma("sp", ev[:], dram["s5_evec"], writes=[Res("ev")])
            S.dma("sp", msk[:], dram["s5_mask4"], writes=[Res("msk")])
            S.barrier()
            sm_ = lambda n: self.sb(s2, [128, NG], F32, n)[0]
            dt, adt, th, t8, k8, lm1, den, zr, zi, ta, tb_ = [sm_(n) for n in "dt adt th t8 k8 lm1 den zr zi ta tb".split()]
            k8i, _ = self.sb(s2, [128, NG], I32, "k8i")
            big = lambda n: self.sb(s2, [128, NG, 32], F32, n)[0]
            angE, magE, cosE, sinE, Pr, Pi, kfE = [big(n) for n in "angE magE cosE sinE Pr Pi kfE".split()]
            kiE, _ = self.sb(s2, [128, NG, 32], I32, "kiE")
            R = [Rp]
            ar, ai = prm[:, :, 0], prm[:, :, 1]
            self.act(dt[:], prm[:, :, 2], AF.Exp, R, R)
            self.tt(adt[:], ar, dt[:], M, R, R)
            self.tt(th[:], ai, dt[:], M, R, R)
            b3 = lambda a: a.unsqueeze(2).to_broadcast([128, NG, 32])
            evb = ev[:].unsqueeze(1).to_broadcast([128, NG, 32])
            self.tt(angE[:], b3(th[:]), evb, M, R, R)
            self.tt(magE[:], b3(adt[:]), evb, M, R, R)
            self.act(magE[:], magE[:], AF.Exp, R, R)
            self.sin_of(cosE[:], angE[:], math.pi / 2, kfE[:], kiE[:], Rp)
            self.sin_of(sinE[:], angE[:], 0.0, kfE[:], kiE[:], Rp)
            self.tt(Pr[:], magE[:], cosE[:], M, R, R)
            self.tt(Pi[:], magE[:], sinE[:], M, R, R)
            self.act(rho[:], adt[:], AF.Exp, R, R, scale=8.0)
            self.tsc(t8[:], th[:], 8.0, None, M, None, R, R)
            self.tsc(k8[:], t8[:], 1.0 / (2 * math.pi), None, M, None, R, R)
            self.cp(k8i[:], k8[:], R, R)
            self.cp(k8[:], k8i[:], R, R)
            self.stt(phi[:], k8[:], -6.28125, t8[:], M, A_, R, R)
            self.stt(phi[:], k8[:], -(2 * math.pi - 6.28125), phi[:], M, A_, R, R)
            lr, li = Pr[:, :, 9], Pi[:, :, 9]
            self.tsc(lm1[:], lr, -1.0, None, A_, None, R, R)
            self.tt(den[:], ar, ar, M, R, R)
            self.tt(ta[:], ai, ai, M, R, R)
            self.tt(den[:], den[:], ta[:], A_, R, R)
            self.recip(den[:], den[:], R, R)
            self.tt(ta[:], lm1[:], ar, M, R, R)
            self.tt(tb_[:], li, ai, M, R, R)
            self.tt(zr[:], ta[:], tb_[:], A_, R, R)
            self.tt(zr[:], zr[:], den[:], M, R, R)
            self.tt(ta[:], li, ar, M, R, R)
            self.tt(tb_[:], lm1[:], ai, M, R, R)
            self.tt(zi[:], ta[:], tb_[:], SUB, R, R)
            self.tt(zi[:], zi[:], den[:], M, R, R)
            v16 = lambda n: self.sb(s2, [128, NG, 16], F32, n)[0]
            Bbr, Bbi, u1, u2 = [v16(n) for n in "Bbr Bbi u1 u2".split()]
            Br, Bi = prm[:, :, 3:19], prm[:, :, 19:35]
            Cr, Ci = prm[:, :, 35:51], prm[:, :, 51:67]
            b16 = lambda a: a.unsqueeze(2).to_broadcast([128, NG, 16])
            self.tt(u1[:], b16(zr[:]), Br, M, R, R)
            self.tt(u2[:], b16(zi[:]), Bi, M, R, R)
            self.tt(Bbr[:], u1[:], u2[:], SUB, R, R)
            self.tt(u1[:], b16(zr[:]), Bi, M, R, R)
            self.tt(u2[:], b16(zi[:]), Br, M, R, R)
            self.tt(Bbi[:], u1[:], u2[:], A_, R, R)
            w4 = lambda n: self.sb(s2, [128, NG, 8, 16], F32, n)[0]
            X1r, X1i, X2r, X2i, w1, w2 = [w4(n) for n in "X1r X1i X2r X2i w1 w2".split()]

            def cprod(k0, Xr, Xi, outr, outi, neg_i=False):
                pk = lambda P_: P_[:, :, k0:k0 + 8].unsqueeze(3).to_broadcast([128, NG, 8, 16])
                xb = lambda X: X.unsqueeze(2).to_broadcast([128, NG, 8, 16])
                self.tt(w1[:], pk(Pr), xb(Xr), M, R, R)
                self.tt(w2[:], pk(Pi), xb(Xi), M, R, R)
                self.tt(outr[:], w1[:], w2[:], SUB, R, R)
                self.tt(w1[:], pk(Pr), xb(Xi), M, R, R)
                self.tt(w2[:], pk(Pi), xb(Xr), M, R, R)
                if neg_i:
                    self.stt(outi[:], w1[:], -1.0, w2[:], M, SUB, R, R)
                else:
                    self.tt(outi[:], w1[:], w2[:], A_, R, R)
            fl = lambda X, rows, g8: X[rows, g8].rearrange("p a b -> p (a b)")
            cprod(0, Bbr[:], Bbi[:], X1r, X1i, neg_i=True)
            cprod(8, Cr, Ci, X2r, X2i)
            for ll in range(NL):
                for g4 in range(4):
                    pt, pr = self.psum()
                    for j in range(4):
                        g = g4 * 4 + j
                        rows, g8 = slice((g % 2) * 64, (g % 2) * 64 + 64), ll * 8 + g // 2
                        self.mm(pt[:, j * 128:(j + 1) * 128], fl(X1r, rows, g8), fl(X2r, rows, g8), True, False, R, [pr])
                        self.mm(pt[:, j * 128:(j + 1) * 128], fl(X1i, rows, g8), fl(X2i, rows, g8), False, True, R, [pr])
                    self.tt(Tg[:, ll, g4 * 4:(g4 + 1) * 4, :], pt[:, :].rearrange("p (a b) -> p a b", b=128),
                            msk[:].rearrange("p (a b) -> p a b", b=128), M, [pr], [RTg])
            S.barrier()
            cprod(16, Cr, Ci, X2r, X2i, neg_i=True)
            self.cp(Rpr[:], X2r[:].rearrange("p g a b -> p g (a b)"), R, [RRp])
            self.cp(Rpi[:], X2i[:].rearrange("p g a b -> p g (a b)"), R, [RRp])
            cprod(24, Bbr[:], Bbi[:], X1r, X1i)
            for ll in range(NL):
                for half in range(2):
                    for ri, X in enumerate((X1r, X1i)):
                        pt, pr = self.psum()
                        for j in range(8):
                            g = half * 8 + j
                            rows, g8 = slice((g % 2) * 64, (g % 2) * 64 + 64), ll * 8 + g // 2
                            self.tr(pt[:, j * 64:(j + 1) * 64], fl(X, rows, g8), self.c_ident_f[rows, rows], R, [pr])
                        self.cp(G[:, ll, half * 8:(half + 1) * 8, ri, :], pt[:, :].rearrange("p (a b) -> p a b", b=64), [pr], [RG])
            S.barrier()
            for nm, t in (("Tg", Tg), ("G", G), ("Rpr", Rpr), ("Rpi", Rpi), ("rho", rho), ("phi", phi)):
                S.dma("sp", self.s5scr[nm], t[:], reads=[RTg, RG, RRp, Rp], writes=[Res("st_" + nm)])
            S.barrier()
            s2.close()
            with ExitStack() as s3:
                cidx, _ = self.sb(s3, [128, 256], F32, "cidx")
                S.dma("sp", cidx[:], dram["s5_cidx"], writes=[Res("cidx")])
                S.barrier()
                HG = NG // 2
                ang2, Rq = self.sb(s3, [128, HG, 256], F32, "ang2")
                kf2, _ = self.sb(s3, [128, HG, 256], F32, "kf2")
                ki2, _ = self.sb(s3, [128, HG, 256], I32, "ki2")
                Ec2, _ = self.sb(s3, [128, HG, 256], F32, "Ec2")
                Es2, _ = self.sb(s3, [128, HG, 256], F32, "Es2")
                for hf in range(2):
                    gsl = slice(hf * HG, (hf + 1) * HG)
                    self.tt(ang2[:], phi[:, gsl].unsqueeze(2).to_broadcast([128, HG, 256]),
                            cidx[:].unsqueeze(1).to_broadcast([128, HG, 256]), M, [Rp, Rq], [Rq])
                    self.sin_of(Ec2[:], ang2[:], math.pi / 2, kf2[:], ki2[:], Rq)
                    self.sin_of(Es2[:], ang2[:], 0.0, kf2[:], ki2[:], Rq)
                    S.dma("sp", self.s5scr["Ec"][:, gsl, :], Ec2[:], reads=[Rq], writes=[Res("st_Ec%d" % hf)])
                    S.dma("act", self.s5scr["Es"][:, gsl, :], Es2[:], reads=[Rq], writes=[Res("st_Es%d" % hf)])
                S.barrier()

    def s5(self, l, hT, Rh, OUT, ROUT):
        S, dram = self.S, self.dram
        M, A_, SUB = ALU.mult, ALU.add, ALU.subtract
        with ExitStack() as st:
            Tg, RTg = self.sb(st, [128, 16, 128], BF16, "Tg")
            G, RG = self.sb(st, [128, 16, 2, 64], BF16, "G")
            Rpr, RRp = self.sb(st, [128, 8, 128], BF16, "Rpr")
            Rpi, _ = self.sb(st, [128, 8, 128], BF16, "Rpi")
            rho, Rp = self.sb(st, [128, 8], F32, "rho")
            phi, _ = self.sb(st, [128, 8], F32, "phi")
            S.dma("sp", Tg[:], self.s5scr["Tg"][:, l], writes=[RTg])
            S.dma("act", G[:], self.s5scr["G"][:, l], writes=[RG])
            S.dma("sp", Rpr[:], self.s5scr["Rpr"][:, l * 8:(l + 1) * 8], writes=[RRp])
            S.dma("act", Rpi[:], self.s5scr["Rpi"][:, l * 8:(l + 1) * 8], writes=[Res("ld_rpi")])
            S.dma("sp", rho[:], self.s5scr["rho"][:, l * 8:(l + 1) * 8], writes=[Rp])
            S.dma("act", phi[:], self.s5scr["phi"][:, l * 8:(l + 1) * 8], writes=[Res("ld_phi")])
            S.barrier()
            UC, RUC = self.sb(st, [128, 2, 16, 8, 16], BF16, "UC")
            UG, RUG = self.sb(st, [128, 16, 256], BF16, "UG")
            Sre, RS = self.sb(st, [128, 8, 257], BF16, "Sre")
            Sim, _ = self.sb(st, [128, 8, 257], BF16, "Sim")
            wdst = ExitStack()
            wd, Rwd = self.load_w(wdst, dram["W_D"][l], [128, 8, 256], "wd")
            k = 0
            for cb in range(2):
                for i2 in range(4):
                    pt, pr = self.psum()
                    for ii in range(2):
                        i = i2 * 2 + ii
                        for kt in range(8):
                            lhs = hT[:, kt, cb * 1024:(cb + 1) * 1024].rearrange("p (c i) -> p c i", i=8)[:, :, i]
                            self.mm(pt[:, ii * 256:(ii + 1) * 256], lhs, wd[:, kt, :], kt == 0, kt == 7, [Rwd, Rh[cb * 2], Rh[cb * 2 + 1]], [pr])
                    dst = UC[:, cb, :, i2 * 2:(i2 + 1) * 2, :]
                    src = pt[:, :].rearrange("p (ii g h) -> p g ii h", ii=2, g=16, h=16)
                    if k % 2 == 0:
                        self.act(dst, src, AF.Copy, [pr], [RUC])
                    else:
                        self.cp(dst, src, [pr], [RUC])
                    k += 1
            S.barrier()
            wdst.close()
            for cb in range(2):
                for half in range(2):
                    pt, pr = self.psum()
                    ptb = pt[:, :].bitcast(BF16)
                    for j in range(8):
                        g = half * 8 + j
                        self.tr(ptb[:, j * 128:(j + 1) * 128], UC[:, cb, g].rearrange("p i h -> p (i h)"), self.c_ident[:], [RUC], [pr])
                    self.cp(UG[:, half * 8:(half + 1) * 8, cb * 128:(cb + 1) * 128], ptb.rearrange("p (a b) -> p a b", b=128), [pr], [RUG])
            self.memset(Sre[:, :, 0:1], 0.0, [RS])
            self.memset(Sim[:, :, 0:1], 0.0, [RS])
            with ExitStack() as s2:
                cidx, _ = self.sb(s2, [128, 256], F32, "cidx")
                cmask, _ = self.sb(s2, [128, 256], F32, "cmask")
                S.dma("sp", cidx[:], dram["s5_cidx"], writes=[Res("cidx")])
                S.dma("sp", cmask[:], dram["s5_cmask"], writes=[Res("cmask")])
                S.barrier()
                q2 = lambda n, dt_=F32: self.sb(s2, [128, 2, 256], dt_, n)[0]
                Ec, Es, rmask, Zr, Zi, t1, t2, t3, t4 = [q2(n) for n in "Ec Es rmask Zr Zi t1 t2 t3 t4".split()]
                RZ = Res("s5z")
                REs = Res("s5es")
                RZl = [RZ, REs]
                for qd in range(4):
                    gs = slice(qd * 2, qd * 2 + 2)
                    bq = lambda a: a.unsqueeze(2).to_broadcast([128, 2, 256])
                    cb_ = lambda a: a.unsqueeze(1).to_broadcast([128, 2, 256])
                    g0 = l * 8 + qd * 2
                    S.dma("sp", Ec[:], self.s5scr["Ec"][:, g0:g0 + 2, :], reads=[RZ], writes=[RZ])
                    S.dma("act", Es[:], self.s5scr["Es"][:, g0:g0 + 2, :], reads=[RZ], writes=[REs])
                    self.tt(rmask[:], bq(rho[:, gs]), cb_(cmask[:]), M, [Rp, RZ], RZl)
                    for gg in range(2):
                        g8 = qd * 2 + gg
                        pz, rpz = self.psum()
                        for g2 in range(2):
                            g = g8 * 2 + g2
                            rows = slice(g2 * 64, g2 * 64 + 64)
                            self.mm(pz[rows, 0:256], G[:, g, 0, :], UG[:, g, :], True, True, [RG, RUG], [rpz])
                            self.mm(pz[rows, 256:512], G[:, g, 1, :], UG[:, g, :], True, True, [RG, RUG], [rpz])
                        self.act(Zr[:, gg, :], pz[:, 0:256], AF.Copy, [rpz], RZl)
                        self.act(Zi[:, gg, :], pz[:, 256:512], AF.Copy, [rpz], RZl)
                    self.tt(t1[:], Ec[:], Zr[:], M, RZl, RZl)
                    self.tt(t2[:], Es[:], Zi[:], M, RZl, RZl)
                    self.tt(t3[:], Ec[:], Zi[:], M, RZl, RZl)
                    self.tt(t4[:], Es[:], Zr[:], M, RZl, RZl)
                    self.tt(Zr[:], t1[:], t2[:], A_, RZl, RZl)
                    self.tt(Zi[:], t3[:], t4[:], SUB, RZl, RZl)
                    fz = lambda a: a[:].rearrange("p a b -> p (a b)")
                    self.scan(fz(Zr), fz(rmask), fz(Zr), 0.0, M, A_, RZl, RZl)
                    self.scan(fz(Zi), fz(rmask), fz(Zi), 0.0, M, A_, RZl, RZl)
                    self.tt(t1[:], Ec[:], Zr[:], M, RZl, RZl)
                    self.tt(t2[:], Es[:], Zi[:], M, RZl, RZl)
                    self.tt(t3[:], Ec[:], Zi[:], M, RZl, RZl)
                    self.tt(t4[:], Es[:], Zr[:], M, RZl, RZl)
                    self.tt(Sre[:, gs, 1:257], t1[:], t2[:], SUB, RZl, [RS])
                    self.tt(Sim[:, gs, 1:257], t3[:], t4[:], A_, RZl, [RS])
                S.barrier()
            with ExitStack() as s2:
                Dt, RD = self.sb(s2, [128, 256], F32, "Dt")
                S.dma("sp", Dt[:], dram["S5D"][l], writes=[RD])
                wglu, Rwglu = self.load_w(s2, dram["W_GLU"][l], [128, 2, 256], "wglu")
                DU, RDU = self.sb(s2, [128, 16, 8, 16], F32, "DU")
                Yf, RYf = self.sb(s2, [128, 16, 8, 16], F32, "Yf")
                Yb, RYb = self.sb(s2, [128, 8, 16, 16], BF16, "Yb")
                yT, _ = self.sb(s2, [128, 2, T], BF16, "yT")
                RyT = [Res("yT0"), Res("yT1")]
                sg, Rsg = self.sb(s2, [128, 512], F32, "sgl")
                for cb in range(2):
                    self.tt(DU[:], UC[:, cb], Dt[:].rearrange("p (g h) -> p g h", h=16).unsqueeze(2).to_broadcast([128, 16, 8, 16]), M,
                            [RUC, RD], [RDU])
                    for g4 in range(4):
                        pt, pr = self.psum()
                        for j in range(4):
                            g = g4 * 4 + j
                            rows, g8 = slice((g % 2) * 64, (g % 2) * 64 + 64), g // 2
                            cs_ = slice(cb * 128, (cb + 1) * 128)
                            o_ = pt[:, j * 128:(j + 1) * 128]
                            self.mm(o_, UG[:, g, cs_], Tg[:, g, :], True, False, [RUG, RTg], [pr])
                            self.mm(o_, Sre[rows, g8, cs_], Rpr[rows, g8, :], False, False, [RS, RRp], [pr])
                            self.mm(o_, Sim[rows, g8, cs_], Rpi[rows, g8, :], False, True, [RS, RRp], [pr])
                        gsl = slice(g4 * 4, (g4 + 1) * 4)
                        self.tt(Yf[:, gsl], pt[:, :].rearrange("c (g j h) -> c g j h", j=8, h=16), DU[:, gsl], A_, [pr, RDU], [RYf])
                    self.act(Yb[:].rearrange("c j g h -> c g j h"), Yf[:], AF.Gelu_apprx_tanh, [RYf], [RYb])
                    for half in range(2):
                        pt, pr = self.psum()
                        ptb = pt[:, :].bitcast(BF16)
                        for j in range(8):
                            self.tr(ptb[:, j * 128:(j + 1) * 128], Yb[:, j].rearrange("c g h -> c (g h)")[:, half * 128:(half + 1) * 128], self.c_ident[:], [RYb], [pr])
                        dst = yT[:, half, cb * 1024:(cb + 1) * 1024].rearrange("p (c j) -> p j c", j=8)
                        self.cp(dst, ptb.rearrange("p (j c) -> p j c", c=128), [pr], [RyT[cb]])
                self.tap("yT", yT[:], RyT[1], [128, 2, T], BF16)
                for tb in range(4):
                    ts = slice(tb * 512, (tb + 1) * 512)
                    for mt in range(2):
                        pt, pr = self.psum()
                        for kt in range(2):
                            self.mm(pt[:, :], wglu[:, kt, mt * 128:(mt + 1) * 128], yT[:, kt, ts], kt == 0, kt == 1, [Rwglu, RyT[tb // 2]], [pr])
                        self.act(sg[:], pt[:, :], AF.Sigmoid, [pr], [Rsg], bias=self.sm(l, "b_glu", mt))
                        self.tt(OUT[:, 3, mt, ts], yT[:, mt, ts], sg[:], M, [RyT[tb // 2], Rsg], [ROUT[3][tb]])
                S.barrier()
            S.barrier()


DRAM_SHAPES = {
    "xT": ([128, 8, T], F32), "memT": ([128, 8, MEM], F32), "pos": ([1, T], I32),
    "ident": ([128, 128], F32), "ones": ([128, 128], F32), "ones_bd": ([128, 128], F32), "negm": ([128, 128], F32),
    "gla_mask": ([128, 512], F32), "ropec": ([128, 2], F32), "reset64": ([128, T], F32), "s5_evec": ([128, 32], F32), "s5_cidx": ([128, 256], F32),
    "s5_mask4": ([128, 512], F32), "s5_cmask": ([128, 256], F32),
}


def build_program(weights, consts, stages=("mix", "xa", "ffn"), nlayers=NL, taps=(), branches="abcdm"):
    nc = bass.Bass("TRN2", target_bir_lowering=False)
    dram = {}
    for k, (shape, dt) in DRAM_SHAPES.items():
        dram[k] = nc.dram_tensor(k, shape, dt, kind="ExternalInput").ap()
    for k, v in weights.items():
        dram[k] = nc.dram_tensor(k, list(v.shape), F32, kind="ExternalInput").ap()
    dram["outT"] = nc.dram_tensor("outT", [128, 8, T], F32, kind="ExternalOutput").ap()
    with ExitStack() as es:
        S = Sched(nc, es)
        kb = KB(nc, es, S, dram, taps)
        kb.branches = branches
        kb.setup(es, pre_x=(kb.s5_params_all if ("mix" in stages and "d" in branches) else None))
        for l in range(nlayers):
            if "mix" in stages:
                kb.mixer(l)
            if "xa" in stages:
                kb.xattn(l)
            if "ffn" in stages:
                kb.ffn(l)
        kb.store_out()
        S.emit()
    return nc


_CACHE = {}


def kernel(**inputs):
    inp = {k: np.asarray(v) for k, v in inputs.items()}
    weights = prep_weights(inp)
    consts = make_consts()
    nc = build_program(weights, consts)
    in_maps = []
    for b in range(8):
        m = {}
        m["xT"] = np.ascontiguousarray(inp["x"][b].T.reshape(8, 128, T).transpose(1, 0, 2))
        m["memT"] = np.ascontiguousarray(inp["mem"][b].T.reshape(8, 128, MEM).transpose(1, 0, 2))
        m["pos"] = np.ascontiguousarray(inp["positions"][b].reshape(1, T).astype(np.int32))
        m.update(consts)
        m.update(weights)
        in_maps.append(m)
    res = run_bass_kernel_spmd(nc, in_maps, core_ids=list(range(8)))
    out = np.empty((8, T, D), np.float32)
    for b in range(8):
        o = res.results[b]["outT"]
        out[b] = o.transpose(2, 1, 0).reshape(T, D)
    return out
```
